# Optimizing a Trainium2 kernel written in Bass

```python
import math
import jax
import jax.numpy as jnp
from jax import lax
import numpy as np

D_MODEL = 1024
BATCH = 4
SEQ = 4096
DEPTH = 2

GRID_W = 64
CTX_LEN = 256
GLA_HEADS = 4
GLA_DK = 64
GLA_DV = 128
GLA_KEY = GLA_HEADS * GLA_DK
GLA_VAL = GLA_HEADS * GLA_DV
GLA_RANK = 16
GLA_GATE_NORM = 16.0
GLA_CHUNK = 64
S5_WIDTH = 512
S5_GROUP = 16
S5_GROUPS = S5_WIDTH // S5_GROUP
S5_STATE = 64
FFN_DENSE = 2816
N_EXPERTS = 8
TOP_K = 2
FFN_EXPERT = 3584
N_DENSE = (DEPTH + 1) // 2
N_MOE = DEPTH // 2
EPS = 1e-6
IN_SIZES = (GLA_KEY, GLA_VAL, GLA_RANK, GLA_RANK, S5_WIDTH, GLA_KEY, GLA_VAL, D_MODEL, D_MODEL)
IN_OFFSETS = tuple(int(o) for o in np.cumsum(IN_SIZES)[:-1])
IN_WIDTH = int(sum(IN_SIZES))
STATE_WIDTH = GLA_KEY + GLA_VAL + 2 * GLA_RANK + S5_WIDTH

kernel_name = 'hybrid_gla_s5_moe_prefix_dit'


def _rmsnorm(x, w):
    xf = x.astype(jnp.float32)
    y = xf * lax.rsqrt(jnp.mean(xf * xf, axis=-1, keepdims=True) + EPS)
    return (y * w.astype(jnp.float32)).astype(x.dtype)


def _modulate(h, shift, scale):
    return h * (1 + scale) + shift


def _flip(t):
    return jnp.flip(t, axis=1)


def _to_col(t, rows):
    b_, n, ch = t.shape
    return t.reshape(b_, rows, GRID_W, ch).transpose(0, 2, 1, 3).reshape(b_, n, ch)


def _to_row(t, rows):
    b_, n, ch = t.shape
    return t.reshape(b_, GRID_W, rows, ch).transpose(0, 2, 1, 3).reshape(b_, n, ch)


def _gla_chunked(q, k, v, g, s0):
    b_, n, h, dk = q.shape
    dv = v.shape[-1]
    nc = n // GLA_CHUNK
    q, k, v, g = (t.reshape(b_, nc, GLA_CHUNK, h, t.shape[-1]) for t in (q, k, v, g))
    bcum = jnp.cumsum(g, axis=2)
    blast = bcum[:, :, -1:]
    qd = q * jnp.exp(bcum)
    kd = k * jnp.exp(-bcum)
    kl = k * jnp.exp(blast - bcum)
    mask = jnp.tril(jnp.ones((GLA_CHUNK, GLA_CHUNK), bool))
    att = jnp.where(mask, jnp.einsum('bnihd,bnjhd->bnhij', qd, kd), 0.0)
    o = jnp.einsum('bnhij,bnjhe->bnihe', att, v)
    ds = jnp.einsum('bnjhd,bnjhe->nbhde', kl, v)
    decay = jnp.exp(blast[:, :, 0]).transpose(1, 0, 2, 3)

    def step(s, inp):
        d, dsn = inp
        return d[..., None] * s + dsn, s

    s_fin, s_start = lax.scan(step, s0, (decay, ds))
    o = o + jnp.einsum('bnihd,nbhde->bnihe', qd, s_start)
    return o.reshape(b_, n, h, dv), s_fin


def _gla_final_state(k, v, g):
    btot = jnp.cumsum(g, axis=1)
    kl = k * jnp.exp(btot[:, -1:] - btot)
    return jnp.einsum('blhd,blhe->bhde', kl, v)


def _gla_bidir(q, k, v, gf, gb, sf0, sb0):
    of, sf = _gla_chunked(q, k, v, gf, sf0)
    ob, sb = _gla_chunked(_flip(q), _flip(k), _flip(v), _flip(gb), sb0)
    return of + _flip(ob), sf, sb


def _s5_discretize(a_re, a_im, log_dt, b_re, b_im):
    f32 = jnp.float32
    lam = lax.complex(a_re.astype(f32), a_im.astype(f32))
    delta = jnp.exp(log_dt.astype(f32))[:, None]
    lam_bar = jnp.exp(lam * delta)
    b_mat = lax.complex(b_re.astype(f32), b_im.astype(f32))
    b_bar = ((lam_bar - 1) / lam)[..., None] * b_mat
    return lam_bar, b_bar


def _linear_combine(e1, e2):
    a1, b1 = e1
    a2, b2 = e2
    return a1 * a2, a2 * b1 + b2


def _s5_scan(u, lam_bar, b_bar, h0):
    bu = jnp.einsum('blgh,gph->blgp', u.astype(jnp.float32).astype(jnp.complex64), b_bar)
    a = jnp.broadcast_to(lam_bar, (1, u.shape[1]) + lam_bar.shape)
    a_cum, h = lax.associative_scan(_linear_combine, (a, bu), axis=1)
    if h0 is not None:
        h = h + a_cum * h0[:, None]
    return h


def _s5_bidir(u, disc_f, disc_b, h0f, h0b):
    b_, n, _ = u.shape
    ug = u.reshape(b_, n, S5_GROUPS, S5_GROUP)
    hf = _s5_scan(ug, *disc_f, h0f)
    hb = _flip(_s5_scan(_flip(ug), *disc_b, h0b))
    return hf, hb


def _s5_readout(hf, hb, u, c_mat, d_skip):
    b_, n, _ = u.shape
    y = jnp.real(jnp.einsum('blgp,ghp->blgh', hf + hb, c_mat)).reshape(b_, n, S5_WIDTH)
    return y.astype(u.dtype) + d_skip * u


def _state_inputs(k, v, zf, zb, u, p):
    b_, n, _ = k.shape
    gf = jax.nn.log_sigmoid((zf @ p['gla_lr_f'] + p['gla_bias_f']).astype(jnp.float32)) / GLA_GATE_NORM
    gb = jax.nn.log_sigmoid((zb @ p['gla_lr_b'] + p['gla_bias_b']).astype(jnp.float32)) / GLA_GATE_NORM
    hd = lambda t, d: t.reshape(b_, n, GLA_HEADS, d)
    return {'k': hd(k, GLA_DK), 'v': hd(v, GLA_DV), 'gf': hd(gf, GLA_DK), 'gb': hd(gb, GLA_DK), 'u': u}


def _project(h, p, full):
    b_, n, _ = h.shape
    if full:
        k, v, zf, zb, u, q, r, ga, gm = jnp.split(h @ p['w_in'], IN_OFFSETS, axis=-1)
        d = _state_inputs(k, v, zf, zb, u, p)
        d.update(q=q.reshape(b_, n, GLA_HEADS, GLA_DK) * GLA_DK ** -0.5, r=r, ga=ga, gm=gm)
        return d
    k, v, zf, zb, u = jnp.split(h @ p['w_in'][:, :STATE_WIDTH], IN_OFFSETS[:4], axis=-1)
    return _state_inputs(k, v, zf, zb, u, p)


def _merge(o_gla, y_s5, pr, p):
    b_, n = y_s5.shape[:2]
    dt = y_s5.dtype
    o = o_gla * lax.rsqrt(jnp.mean(o_gla * o_gla, axis=-1, keepdims=True) + EPS) * p['gla_norm_w'].astype(jnp.float32)
    o = o.reshape(b_, n, GLA_VAL).astype(dt) * jax.nn.silu(pr['r'])
    ya = o @ p['w_gla_proj']
    s = jax.nn.gelu(y_s5)
    s = s * jax.nn.sigmoid(s @ p['s5_w_glu'] + p['s5_b_glu'])
    yb = s @ p['w_s5_proj']
    m = jax.nn.sigmoid(pr['ga']) * ya + jax.nn.sigmoid(pr['gm']) * yb
    return m @ p['w_out']


def _token_mixer(hl, hc, p, rows, ctx_out):
    disc_f = _s5_discretize(p['s5_a_re_f'], p['s5_a_im_f'], p['s5_log_dt_f'], p['s5_b_re'], p['s5_b_im'])
    disc_b = _s5_discretize(p['s5_a_re_b'], p['s5_a_im_b'], p['s5_log_dt_b'], p['s5_b_re'], p['s5_b_im'])
    c_mat = lax.complex(p['s5_c_re'].astype(jnp.float32), p['s5_c_im'].astype(jnp.float32))
    pc = _project(hc, p, ctx_out)
    hcf, hcb = _s5_bidir(pc['u'], disc_f, disc_b, None, None)
    if ctx_out:
        s0 = jnp.zeros((hc.shape[0], GLA_HEADS, GLA_DK, GLA_DV), jnp.float32)
        oc, sfc, sbc = _gla_bidir(pc['q'], pc['k'], pc['v'], pc['gf'], pc['gb'], s0, s0)
        out_c = _merge(oc, _s5_readout(hcf, hcb, pc['u'], c_mat, p['s5_d']), pc, p)
    else:
        sfc = _gla_final_state(pc['k'], pc['v'], pc['gf'])
        sbc = _gla_final_state(_flip(pc['k']), _flip(pc['v']), _flip(pc['gb']))
        out_c = None
    pl = _project(hl, p, True)
    ol, _, _ = _gla_bidir(pl['q'], pl['k'], pl['v'], pl['gf'], pl['gb'], sfc, sbc)
    ul = _to_col(pl['u'], rows)
    hlf, hlb = _s5_bidir(ul, disc_f, disc_b, hcf[:, -1], hcb[:, 0])
    yl = _to_row(_s5_readout(hlf, hlb, ul, c_mat, p['s5_d']), rows)
    out_l = _merge(ol, yl, pl, p)
    return out_l, out_c


def _swiglu(h, w_gate, w_up, w_down):
    return (jax.nn.silu(h @ w_gate) * (h @ w_up)) @ w_down


def _moe(h, w_router, w_gate, w_up, w_down):
    logits = (h @ w_router).astype(jnp.float32)
    top_v, top_i = lax.top_k(logits, TOP_K)
    weights = jax.nn.softmax(top_v, axis=-1)
    gates = jnp.sum(jax.nn.one_hot(top_i, N_EXPERTS, dtype=jnp.float32) * weights[..., None], axis=-2).astype(h.dtype)
    out = jnp.zeros_like(h)
    for e in range(N_EXPERTS):
        out = out + gates[..., e:e + 1] * _swiglu(h, w_gate[e], w_up[e], w_down[e])
    return out


def setup_inputs(seed: int = 0) -> dict:
    key = jax.random.key(seed)
    ks = iter(jax.random.split(key, 64))
    f32 = jnp.float32
    nrm = lambda shape, scale: scale * jax.random.normal(next(ks), shape, f32)
    D, G, P, H, L_ = D_MODEL, S5_GROUPS, S5_STATE, S5_GROUP, DEPTH
    n_idx = jnp.arange(P, dtype=f32)
    log_dt = lambda: jax.random.uniform(next(ks), (L_, G), f32, math.log(1e-3), math.log(1e-1))
    return {
        'x': nrm((BATCH, SEQ, D), 1.0),
        'c': nrm((BATCH, D), 1.0),
        'ctx': nrm((BATCH, CTX_LEN, D), 1.0),
        'c_ctx': nrm((D,), 1.0),
        'w_mod': nrm((L_, D, 6 * D), 0.5 * D ** -0.5),
        'b_mod': nrm((L_, 6 * D), 0.01),
        'norm1_w': 1.0 + nrm((L_, D), 0.02),
        'norm2_w': 1.0 + nrm((L_, D), 0.02),
        'final_norm_w': 1.0 + nrm((D,), 0.02),
        'w_in': nrm((L_, D, IN_WIDTH), D ** -0.5),
        'gla_lr_f': nrm((L_, GLA_RANK, GLA_KEY), GLA_RANK ** -0.5),
        'gla_lr_b': nrm((L_, GLA_RANK, GLA_KEY), GLA_RANK ** -0.5),
        'gla_bias_f': nrm((L_, GLA_KEY), 0.1),
        'gla_bias_b': nrm((L_, GLA_KEY), 0.1),
        'gla_norm_w': 1.0 + nrm((L_, GLA_DV), 0.02),
        's5_a_re_f': -0.5 + nrm((L_, G, P), 0.01),
        's5_a_im_f': math.pi * n_idx + nrm((L_, G, P), 0.01),
        's5_log_dt_f': log_dt(),
        's5_a_re_b': -0.5 + nrm((L_, G, P), 0.01),
        's5_a_im_b': math.pi * n_idx + nrm((L_, G, P), 0.01),
        's5_log_dt_b': log_dt(),
        's5_b_re': nrm((L_, G, P, H), (2 * H) ** -0.5),
        's5_b_im': nrm((L_, G, P, H), (2 * H) ** -0.5),
        's5_c_re': nrm((L_, G, H, P), 2 ** -0.5),
        's5_c_im': nrm((L_, G, H, P), 2 ** -0.5),
        's5_d': nrm((L_, S5_WIDTH), 1.0),
        's5_w_glu': nrm((L_, S5_WIDTH, S5_WIDTH), S5_WIDTH ** -0.5),
        's5_b_glu': nrm((L_, S5_WIDTH), 0.01),
        'w_gla_proj': nrm((L_, GLA_VAL, D), GLA_VAL ** -0.5),
        'w_s5_proj': nrm((L_, S5_WIDTH, D), S5_WIDTH ** -0.5),
        'w_out': nrm((L_, D, D), D ** -0.5),
        'ffn_w_gate': nrm((N_DENSE, D, FFN_DENSE), D ** -0.5),
        'ffn_w_up': nrm((N_DENSE, D, FFN_DENSE), D ** -0.5),
        'ffn_w_down': nrm((N_DENSE, FFN_DENSE, D), FFN_DENSE ** -0.5),
        'moe_router': nrm((N_MOE, D, N_EXPERTS), D ** -0.5),
        'moe_w_gate': nrm((N_MOE, N_EXPERTS, D, FFN_EXPERT), D ** -0.5),
        'moe_w_up': nrm((N_MOE, N_EXPERTS, D, FFN_EXPERT), D ** -0.5),
        'moe_w_down': nrm((N_MOE, N_EXPERTS, FFN_EXPERT, D), FFN_EXPERT ** -0.5),
    }


def reference(x, c, ctx, c_ctx, w_mod, b_mod, norm1_w, norm2_w, final_norm_w, w_in,
              gla_lr_f, gla_lr_b, gla_bias_f, gla_bias_b, gla_norm_w,
              s5_a_re_f, s5_a_im_f, s5_log_dt_f, s5_a_re_b, s5_a_im_b, s5_log_dt_b,
              s5_b_re, s5_b_im, s5_c_re, s5_c_im, s5_d, s5_w_glu, s5_b_glu,
              w_gla_proj, w_s5_proj, w_out,
              ffn_w_gate, ffn_w_up, ffn_w_down,
              moe_router, moe_w_gate, moe_w_up, moe_w_down):
    D = D_MODEL
    rows = x.shape[1] // GRID_W
    silu_c = jax.nn.silu(c)
    silu_cc = jax.nn.silu(c_ctx)
    lat, cx = x, ctx
    for l in range(DEPTH):
        ctx_out = l < DEPTH - 1
        p = {
            'w_in': w_in[l], 'gla_lr_f': gla_lr_f[l], 'gla_lr_b': gla_lr_b[l],
            'gla_bias_f': gla_bias_f[l], 'gla_bias_b': gla_bias_b[l], 'gla_norm_w': gla_norm_w[l],
            's5_a_re_f': s5_a_re_f[l], 's5_a_im_f': s5_a_im_f[l], 's5_log_dt_f': s5_log_dt_f[l],
            's5_a_re_b': s5_a_re_b[l], 's5_a_im_b': s5_a_im_b[l], 's5_log_dt_b': s5_log_dt_b[l],
            's5_b_re': s5_b_re[l], 's5_b_im': s5_b_im[l], 's5_c_re': s5_c_re[l], 's5_c_im': s5_c_im[l],
            's5_d': s5_d[l], 's5_w_glu': s5_w_glu[l], 's5_b_glu': s5_b_glu[l],
            'w_gla_proj': w_gla_proj[l], 'w_s5_proj': w_s5_proj[l], 'w_out': w_out[l],
        }
        mod = (silu_c @ w_mod[l] + b_mod[l])[:, None, :]
        sh1, sc1, g1, sh2, sc2, g2 = jnp.split(mod, 6, axis=-1)
        n_mod = 6 if ctx_out else 2
        mc = jnp.split(silu_cc @ w_mod[l][:, :n_mod * D] + b_mod[l][:n_mod * D], n_mod)

        def channel_mixer(h):
            if l % 2 == 0:
                i = l // 2
                return _swiglu(h, ffn_w_gate[i], ffn_w_up[i], ffn_w_down[i])
            i = l // 2
            return _moe(h, moe_router[i], moe_w_gate[i], moe_w_up[i], moe_w_down[i])

        hl = _modulate(_rmsnorm(lat, norm1_w[l]), sh1, sc1)
        hc = _modulate(_rmsnorm(cx, norm1_w[l]), mc[0], mc[1])
        ml, mcx = _token_mixer(hl, hc, p, rows, ctx_out)
        lat = lat + g1 * ml
        lat = lat + g2 * channel_mixer(_modulate(_rmsnorm(lat, norm2_w[l]), sh2, sc2))
        if ctx_out:
            cx = cx + mc[2] * mcx
            cx = cx + mc[5] * channel_mixer(_modulate(_rmsnorm(cx, norm2_w[l]), mc[3], mc[4]))
    return _rmsnorm(lat, final_norm_w)
```

```python
import math
from contextlib import ExitStack

import numpy as np
import concourse.bass as bass
import concourse.mybir as mybir
from concourse.bass_utils import run_bass_kernel_spmd

F32 = mybir.dt.float32
BF16 = mybir.dt.bfloat16
AF = mybir.ActivationFunctionType
ALU = mybir.AluOpType

D = 1024
KT = 8
NCTX = 256
NLAT = 4096
NTOK = NCTX + NLAT
NTILE = NTOK // 128
H_FFN = 2816
H_MOE = 3584
NEXP = 8
NOWN = 2048
EPS = 1e-6
WIN = 4128
OK_, OV_, OZF, OZB, OU_, OQ_, OR_, OGA, OGM = 0, 256, 768, 784, 800, 1312, 1568, 2080, 3104
NVEC = 137


class Buf:
    __slots__ = ("name", "lws", "rd", "gid")

    def __init__(self, name):
        self.name = name
        self.lws = []
        self.rd = {}
        self.gid = None


class Prog:
    ENGS = ("pe", "act", "dve", "pool", "sp")
    NDMASEM = 12

    def __init__(self, nc, es):
        self.nc = nc
        self.streams = {e: [] for e in self.ENGS}
        self.cnt = {}
        self.known = {e: {} for e in self.ENGS}
        self.sems = {}
        for e in ("pe", "act", "dve", "pool"):
            self.sems[e] = es.enter_context(nc.semaphore("s_" + e))
            self.cnt[e] = 0
        self.dma_rr = {"sp": 0, "pool": 0}
        for q in ("sp", "pool"):
            for i in range(self.NDMASEM):
                s = f"d_{q}{i}"
                self.sems[s] = es.enter_context(nc.semaphore(s))
                self.cnt[s] = 0
        self.last_dma = []
        self.ninst = 0

    def _need(self, eng, stream, seq):
        k = self.known[eng]
        if k.get(stream, -1) >= seq:
            return
        k[stream] = seq
        sem = self.sems[stream]
        val = (seq + 1) * (16 if stream.startswith("d_") else 1)
        self.streams[eng].append(lambda E, sem=sem, val=val: E.wait_ge(sem, val))
        self.ninst += 1

    def _deps(self, eng, reads, writes, gid=None):
        for b in reads:
            for lw in b.lws:
                if not (eng == "pe" and lw[0] == "pe"):
                    self._need(eng, *lw)
        for b in writes:
            if gid is not None and b.gid == gid:
                continue
            for lw in b.lws:
                if lw[0] != eng:
                    self._need(eng, *lw)
            for st, sq in b.rd.items():
                if st != eng:
                    self._need(eng, st, sq)

    def _mark(self, stream, seq, reads, writes, gid=None):
        for b in reads:
            if b.rd.get(stream, -1) < seq:
                b.rd[stream] = seq
        for b in writes:
            if gid is not None and b.gid == gid:
                b.lws.append((stream, seq))
            else:
                b.lws = [(stream, seq)]
                b.rd = {}
                b.gid = gid

    def op(self, eng, fn, reads=(), writes=()):
        self._deps(eng, reads, writes)
        seq = self.cnt[eng]
        self.cnt[eng] = seq + 1
        sem = self.sems[eng]
        self.streams[eng].append(lambda E, fn=fn, sem=sem: fn(E).then_inc(sem, 1))
        self._mark(eng, seq, reads, writes)
        self.ninst += 1

    def dma(self, q, out, in_, reads=(), writes=(), gid=None, **kw):
        slot = self.dma_rr[q]
        self.dma_rr[q] = (slot + 1) % self.NDMASEM
        stream = f"d_{q}{slot}"
        seq = self.cnt[stream]
        if seq > 0:
            self._need(q, stream, seq - 1)
        self._deps(q, reads, writes, gid)
        self.cnt[stream] = seq + 1
        sem = self.sems[stream]
        self.streams[q].append(
            lambda E, out=out, in_=in_, sem=sem, kw=kw: E.dma_start(out=out, in_=in_, **kw).then_inc(sem, 16))
        self._mark(stream, seq, reads, writes, gid)
        self.ninst += 1

    def barrier(self):
        self.nbar = getattr(self, "nbar", 0) + 1
        for e in self.ENGS:
            for st, c in self.cnt.items():
                if c > 0 and not (st == e and e == "sp"):
                    self._need(e, st, c - 1)

    def wait_all_dma(self, eng="sp"):
        for s, c in self.cnt.items():
            if s.startswith("d_") and c > 0:
                self._need(eng, s, c - 1)

    def emit(self):
        nc = self.nc
        with nc.Block() as block:
            @block.sync
            def _(E):
                for t in self.streams["sp"]:
                    t(E)

            @block.tensor
            def _(E):
                for t in self.streams["pe"]:
                    t(E)

            @block.scalar
            def _(E):
                for t in self.streams["act"]:
                    t(E)

            @block.vector
            def _(E):
                for t in self.streams["dve"]:
                    t(E)

            @block.gpsimd
            def _(E):
                for t in self.streams["pool"]:
                    t(E)


class Ring:
    def __init__(self, items):
        self.items = items
        self.i = 0

    def next(self):
        it = self.items[self.i % len(self.items)]
        self.i += 1
        return it


class KB:
    def __init__(self, dbg=(), stop_after=None, nlayers=2):
        self.nc = bass.Bass("TRN2", target_bir_lowering=False)
        self.es = ExitStack()
        self.P = Prog(self.nc, self.es)
        self.dbg = set(dbg)
        self.stop_after = stop_after
        self.nlayers = nlayers
        self.dbufs = {}
        self.ins = {}
        self.uid = 0

    def inp(self, name, shape, dt=F32):
        ap = self.nc.dram_tensor(name, list(shape), dt, kind="ExternalInput").ap()
        self.ins[name] = ap
        return ap

    def scratch(self, name, shape, dt, out=False):
        kind = "ExternalOutput" if (out or name in self.dbg) else "Internal"
        return self.nc.dram_tensor(name, list(shape), dt, kind=kind).ap()

    def db(self, name, idx=0):
        k = (name, idx)
        if k not in self.dbufs:
            self.dbufs[k] = Buf(f"DRAM:{name}:{idx}")
        return self.dbufs[k]

    def sb(self, es, name, shape, dt):
        self.uid += 1
        t = es.enter_context(self.nc.sbuf_tensor(f"{name}_{self.uid}", list(shape), dt))
        return t, Buf(name)

    def sbr(self, es, name, shape, dt, n):
        return Ring([self.sb(es, f"{name}{i}", shape, dt) for i in range(n)])

    def ps(self, es, name, shape, dt=F32):
        self.uid += 1
        t = es.enter_context(self.nc.psum_tensor(f"{name}_{self.uid}", list(shape), dt))
        return t, Buf(name)

    def psr(self, es, name, shape, n, dt=F32):
        return Ring([self.ps(es, f"{name}{i}", shape, dt) for i in range(n)])

    def mm(self, out, lhsT, rhs, start, stop, reads, writes):
        self.P.op("pe", lambda E: E.matmul(out, lhsT=lhsT, rhs=rhs, start=start, stop=stop), reads, writes)

    def tr(self, out, in_, ident, reads, writes):
        self.P.op("pe", lambda E: E.transpose(out, in_, ident), reads, writes)

    def act(self, out, in_, func, reads, writes, **kw):
        self.P.op("act", lambda E: E.activation(out=out, in_=in_, func=func, **kw), reads, writes)

    def tt(self, eng, out, in0, in1, op, reads, writes):
        self.P.op(eng, lambda E: E.tensor_tensor(out=out, in0=in0, in1=in1, op=op), reads, writes)

    def ts(self, eng, out, in0, s1, s2, op0, op1, reads, writes):
        if s2 is None:
            self.P.op(eng, lambda E: E.tensor_scalar(out=out, in0=in0, scalar1=s1, scalar2=None, op0=op0), reads, writes)
        else:
            self.P.op(eng, lambda E: E.tensor_scalar(out=out, in0=in0, scalar1=s1, scalar2=s2, op0=op0, op1=op1),
                      reads, writes)

    def dump(self, name, ap, reads):
        if ("dump_" + name) not in self.dbg:
            return
        if not hasattr(self, "_dumped"):
            self._dumped = set()
        if name in self._dumped:
            return
        self._dumped.add(name)
        d = self.nc.dram_tensor("dump_" + name, list(ap.shape), ap.dtype, kind="ExternalOutput").ap()
        self.dma(d, ap, reads=reads, writes=[self.db("dump_" + name)])

    def rstd(self, out, in_, b):
        self.act(out, in_, AF.Sqrt, [b], [b], bias=EPS, scale=1.0)
        self.P.op("dve", lambda E: E.reciprocal(out=out, in_=out), [b], [b])

    def stt(self, eng, out, in0, scalar, in1, op0, op1, reads, writes):
        self.P.op(eng, lambda E: E.scalar_tensor_tensor(out=out, in0=in0, scalar=scalar, in1=in1, op0=op0, op1=op1),
                  reads, writes)

    def cp(self, eng, out, in_, reads, writes):
        if eng == "act":
            self.P.op("act", lambda E: E.copy(out=out, in_=in_), reads, writes)
        else:
            self.P.op(eng, lambda E: E.tensor_copy(out=out, in_=in_), reads, writes)

    def memset(self, eng, ap, val, writes):
        self.P.op(eng, lambda E: E.memset(ap, val), (), writes)

    def dma(self, out, in_, reads=(), writes=(), q="sp", gid=None, **kw):
        if gid is None and len(writes) == 1 and writes[0].name.startswith("DRAM:") and not writes[0].name.startswith("DRAM:LAT"):
            gid = (writes[0].name, getattr(self.P, "nbar", 0))
        self.P.dma(q, out, in_, reads, writes, gid=gid, **kw)


def build(dbg=(), stop_after=None, nlayers=2, hook=None):
    kb = KB(dbg, stop_after, nlayers)
    nc, P = kb.nc, kb.P
    shapes = {"x": [NLAT, D], "ctx": [NCTX, D], "cc": [128, KT, 2]}
    for n, s_ in [("w_mod", [2, D, 6 * D]), ("b_mod", [2, 6 * D]), ("norm1_w", [2, D]), ("norm2_w", [2, D]),
                 ("final_norm_w", [D]), ("w_in", [2, D, WIN]), ("gla_lr_f", [2, 16, 256]), ("gla_lr_b", [2, 16, 256]),
                 ("gla_bias_f", [2, 256]), ("gla_bias_b", [2, 256]), ("gla_norm_w", [2, 128]),
                 ("s5_a_re_f", [2, 32, 64]), ("s5_a_im_f", [2, 32, 64]), ("s5_log_dt_f", [2, 32]),
                 ("s5_a_re_b", [2, 32, 64]), ("s5_a_im_b", [2, 32, 64]), ("s5_log_dt_b", [2, 32]),
                 ("s5_b_re", [2, 32, 64, 16]), ("s5_b_im", [2, 32, 64, 16]), ("s5_c_re", [2, 32, 16, 64]),
                 ("s5_c_im", [2, 32, 16, 64]), ("s5_d", [2, 512]), ("s5_w_glu", [2, 512, 512]), ("s5_b_glu", [2, 512]),
                 ("w_gla_proj", [2, 512, D]), ("w_s5_proj", [2, 512, D]), ("w_out", [2, D, D]),
                 ("ffn_w_gate", [1, D, H_FFN]), ("ffn_w_up", [1, D, H_FFN]), ("ffn_w_down", [1, H_FFN, D]),
                 ("moe_router", [1, D, NEXP]), ("moe_w_gate", [1, NEXP, D, H_MOE]), ("moe_w_up", [1, NEXP, D, H_MOE]),
                 ("moe_w_down", [1, NEXP, H_MOE, D]),
                 ("k_ident", [128, 128]), ("k_uf", [128, 128]), ("k_ub", [128, 128]), ("k_amf", [128, 128]),
                 ("k_amb", [128, 128]), ("k_mba", [2, 128]), ("k_mbb", [2, 128]), ("k_nvec", [2, NVEC]),
                 ("k_mge", [128, 128]), ("k_mle", [128, 128])]:
        shapes[n] = s_

    class LazyIn(dict):
        def __missing__(self, n):
            ap = kb.inp(n, shapes[n])
            self[n] = ap
            return ap

    I = LazyIn()
    OUT = nc.dram_tensor("out", [NOWN, D], F32, kind="ExternalOutput").ap()

    S = {}
    S["LAT"] = kb.scratch("LAT", [NTOK, D], F32)
    S["V"] = kb.scratch("V", [NTOK, 512], BF16)
    S["GR"] = kb.scratch("GR", [NTOK, 512], BF16)
    S["GAT"] = kb.scratch("GAT", [D, NTOK], BF16)
    S["GMT"] = kb.scratch("GMT", [D, NTOK], BF16)
    S["UTL"] = kb.scratch("UTL", [512, NLAT], BF16)
    S["UTC"] = kb.scratch("UTC", [512, NCTX], BF16)
    S["KD"] = kb.scratch("KD", [NTILE, 2, 128, 512], BF16)
    S["KQT"] = kb.scratch("KQT", [NTILE, 128, 2, 4, 2, 128], BF16)
    S["OGT"] = kb.scratch("OGT", [512, NTOK], BF16)
    S["YTL"] = kb.scratch("YTL", [512, NLAT], F32)
    S["YTC"] = kb.scratch("YTC", [512, NCTX], F32)
    S["H2T"] = kb.scratch("H2T", [D, NTOK], BF16)
    db = kb.db

    es0 = kb.es
    identf, b_identf = kb.sb(es0, "identf", [128, 128], F32)
    identb, b_identb = kb.sb(es0, "identb", [128, 128], BF16)
    ones, b_ones = kb.sb(es0, "ones", [128, 128], F32)
    uf, b_uf = kb.sb(es0, "uf", [128, 128], F32)
    ub, b_ub = kb.sb(es0, "ub", [128, 128], F32)
    amf, b_amf = kb.sb(es0, "amf", [128, 128], F32)
    amb, b_amb = kb.sb(es0, "amb", [128, 128], F32)
    mba, b_mba = kb.sb(es0, "mba", [2, 128], F32)
    mbb, b_mbb = kb.sb(es0, "mbb", [2, 128], F32)
    dec, b_dec = kb.sb(es0, "dec", [128, NTILE, 2, 2, 4], F32)
    modT, b_modT = kb.sb(es0, "modT", [128, 48, 2], F32)
    vpt, b_vpt = kb.sb(es0, "vpt", [128, 76], F32)
    ab, b_ab = kb.sb(es0, "ab", [128, KT, 8], F32)
    gbc, b_gbc = kb.sb(es0, "gbc", [128, 5, D], F32)
    gnw, b_gnw = kb.sb(es0, "gnw", [128, 1], F32)
    scT, b_scT = kb.sb(es0, "scT", [128, KT, 2], F32)
    gates, b_gates = kb.sb(es0, "gates", [128, NOWN // 128, NEXP], F32)

    for t, b, src in [(identf, b_identf, "k_ident"), (uf, b_uf, "k_uf"), (ub, b_ub, "k_ub"), (amf, b_amf, "k_amf"),
                      (amb, b_amb, "k_amb"), (mba, b_mba, "k_mba"), (mbb, b_mbb, "k_mbb")]:
        kb.dma(t[:], I[src], writes=[b])
    kb.cp("dve", identb[:], identf[:], [b_identf], [b_identb])
    kb.memset("dve", ones[:], 1.0, [b_ones])
    kb.dma(scT[:], I["cc"], writes=[b_scT])
    kb.act(scT[:], scT[:], AF.Silu, [b_scT], [b_scT])
    for i in range(NTILE):
        src = I["ctx"][i * 128:(i + 1) * 128, :] if i < 2 else I["x"][(i - 2) * 128:(i - 1) * 128, :]
        kb.dma(S["LAT"][i * 128:(i + 1) * 128, :], src, writes=[db("LAT", i)])

    CC = dict(identf=(identf, b_identf), identb=(identb, b_identb), ones=(ones, b_ones),
              uf=(uf, b_uf), ub=(ub, b_ub), amf=(amf, b_amf), amb=(amb, b_amb),
                                     mba=(mba, b_mba), mbb=(mbb, b_mbb), dec=(dec, b_dec), modT=(modT, b_modT),
                                     vpt=(vpt, b_vpt), ab=(ab, b_ab), gbc=(gbc, b_gbc), gnw=(gnw, b_gnw),
                                     scT=(scT, b_scT), gates=(gates, b_gates))
    for l in range(nlayers):
        layer(kb, I, S, OUT, l, CC)
        if kb.stop_after is not None and kb.stop_after[0] == l:
            break
    if hook is not None:
        hook(kb, CC, S)
    P.wait_all_dma("sp")
    P.emit()
    kb.es.close()
    return kb


def layer(kb, I, S, OUT, l, C):
    last = (l == kb.nlayers - 1) and kb.nlayers == 2
    stop = kb.stop_after[1] if (kb.stop_after is not None and kb.stop_after[0] == l) else None
    phase_mod(kb, I, l, C)
    if stop == "mod":
        return
    phase_a(kb, I, S, l, C)
    if stop == "a":
        return
    phase_g(kb, I, S, l, C)
    if stop == "g":
        return
    phase_s5(kb, I, S, l, C, last)
    if stop == "s5":
        return
    phase_m(kb, I, S, l, C, last)
    if stop == "m":
        return
    phase_f(kb, I, S, OUT, l, C, last)


def phase_mod(kb, I, l, C):
    db = kb.db
    identf, b_identf = C["identf"]
    ones, b_ones = C["ones"]
    modT, b_modT = C["modT"]
    vpt, b_vpt = C["vpt"]
    ab, b_ab = C["ab"]
    gbc, b_gbc = C["gbc"]
    gnw, b_gnw = C["gnw"]
    scT, b_scT = C["scT"]
    with ExitStack() as es:
        vrow, b_vrow = kb.sb(es, "vrow", [76, 128], F32)
        wmr = kb.sbr(es, "wm", [128, KT, 512], F32, 2)
        dg, b_dg = kb.sb(es, "dg", [128, 4, 128], F32)
        pmod, b_pmod = kb.ps(es, "pmod", [128, 96])
        ptr, b_ptr = kb.ps(es, "ptrm", [128, 128])
        pbc = kb.psr(es, "pbc", [128, 512], 2)
        kb.dma(vrow[0:48, :], I["b_mod"][l].rearrange("(j p) -> j p", p=128), writes=[b_vrow])
        kb.dma(vrow[48:56, :], I["norm1_w"][l].rearrange("(j p) -> j p", p=128), writes=[b_vrow])
        kb.dma(vrow[56:64, :], I["norm2_w"][l].rearrange("(j p) -> j p", p=128), writes=[b_vrow])
        kb.dma(vrow[64:68, :], I["s5_b_glu"][l].rearrange("(j p) -> j p", p=128), writes=[b_vrow])
        kb.dma(vrow[68:76, :], I["final_norm_w"].rearrange("(j p) -> j p", p=128), writes=[b_vrow])
        kb.dma(gnw[:], I["gla_norm_w"][l].rearrange("(p o) -> p o", o=1), writes=[b_gnw])
        kb.tr(ptr[:, 0:76], vrow[0:76, :], identf[0:76, 0:76], [b_vrow, b_identf], [b_ptr])
        kb.cp("dve", vpt[:], ptr[:, 0:76], [b_ptr], [b_vpt])
        wm_src = I["w_mod"][l].rearrange("(k p) n -> p k n", p=128)
        for blk in range(12):
            wm, b_wm = wmr.next()
            kb.dma(wm[:], wm_src[:, :, blk * 512:(blk + 1) * 512], writes=[b_wm])
            for jj in range(4):
                j = blk * 4 + jj
                for k in range(KT):
                    kb.mm(pmod[:, 2 * j:2 * j + 2], wm[:, k, jj * 128:(jj + 1) * 128], scT[:, k, :], k == 0, k == KT - 1,
                          [b_wm, b_scT], [b_pmod])
        kb.tt("dve", modT[:], pmod[:].rearrange("p (j w) -> p j w", w=2),
              vpt[:, 0:48].unsqueeze(2).to_broadcast([128, 48, 2]), ALU.add, [b_pmod, b_vpt], [b_modT])
        for w_, (sc_c, sh_c, nw_c) in enumerate([(8, 0, 48), (8, 0, 48), (32, 24, 56), (32, 24, 56)]):
            who = w_ % 2
            kb.stt("dve", ab[:, :, 2 * w_], modT[:, sc_c:sc_c + 8, who], 1.0, vpt[:, nw_c:nw_c + 8], ALU.add, ALU.mult,
                   [b_modT, b_vpt], [b_ab])
            kb.cp("dve", ab[:, :, 2 * w_ + 1], modT[:, sh_c:sh_c + 8, who], [b_modT], [b_ab])
        for gi, (c0, who) in enumerate([(16, 0), (40, 0), (16, 1), (40, 1), (68, -1)]):
            for half in range(2):
                pb, b_pb = pbc.next()
                for jj in range(4):
                    j = half * 4 + jj
                    col = modT[:, c0 + j, who:who + 1] if who >= 0 else vpt[:, c0 + j:c0 + j + 1]
                    kb.ts("dve", dg[:, jj, :], identf[:], col, None, ALU.mult, None,
                          [b_identf, b_modT, b_vpt], [b_dg])
                    kb.mm(pb[:, jj * 128:(jj + 1) * 128], ones[:], dg[:, jj, :], True, True, [b_ones, b_dg], [b_pb])
                kb.cp("act", gbc[:, gi, half * 512:(half + 1) * 512], pb[:], [b_pb], [b_gbc])
        kb.P.barrier()


def phase_a(kb, I, S, l, C):
    db = kb.db
    identf, b_identf = C["identf"]
    uf, b_uf = C["uf"]
    ub, b_ub = C["ub"]
    mba, b_mba = C["mba"]
    mbb, b_mbb = C["mbb"]
    dec, b_dec = C["dec"]
    ab, b_ab = C["ab"]
    with ExitStack() as es:
        win, b_win = kb.sb(es, "win", [128, KT, WIN], BF16)
        wkd, b_wkd = kb.sb(es, "wkd", [128, KT, 512], BF16)
        wqd, b_wqd = kb.sb(es, "wqd", [128, KT, 512], BF16)
        lrd = [kb.sb(es, "lrd%d" % d, [17, 512], F32) for d in range(2)]
        z1 = [kb.sb(es, "z1%d" % d, [17, 512], F32) for d in range(2)]
        xtr = kb.sbr(es, "xt", [128, D], F32, 2)
        ssr = kb.sbr(es, "ss", [128, 2], F32, 4)
        xs, b_xs = kb.sb(es, "xs", [128, 4, D], F32)
        hTr = kb.sbr(es, "hT", [128, KT, 512], BF16, 2)
        kq, b_kq = kb.sb(es, "kq", [128, 8, 512], F32)
        ust, b_ust = kb.sb(es, "ust", [128, 4, 512], BF16)
        gstr = kb.sbr(es, "gst", [128, 8, 512], BF16, 1)
        vstr = kb.sbr(es, "vst", [128, 512], BF16, 2)
        rstr = kb.sbr(es, "rst", [128, 512], BF16, 2)
        sp = [kb.sb(es, "sp%d" % d, [128, 512], F32) for d in range(2)]
        etmp, b_etmp = kb.sb(es, "etmp", [128, 512], F32)
        ektok, b_ektok = kb.sb(es, "ektok", [128, 512], F32)
        kdstr = kb.sbr(es, "kdst", [128, 2, 512], BF16, 1)
        kqstr = kb.sbr(es, "kqst", [128, 2, 4, 2, 128], BF16, 1)
        ekr = kb.sbr(es, "ek", [128, 128], F32, 2)
        eqr = kb.sbr(es, "eq", [128, 128], F32, 2)
        ptr = kb.psr(es, "ptr", [128, 512], 2)
        pfm = kb.psr(es, "pfm", [128, 512], 2)
        ptm = kb.psr(es, "ptm", [128, 512], 2)
        pg = kb.psr(es, "pg", [128, 512], 2)

        wsrc = I["w_in"][l].rearrange("(k p) n -> p k n", p=128)
        for k in range(KT):
            kb.dma(win[:, k, :], wsrc[:, k, :], writes=[b_win], q="pool", gid=("win", l))
        for r in range(2):
            o5 = wkd[:].rearrange("p k (h r d) -> p k h r d", h=4, r=2)[:, :, :, r, :]
            kb.cp("pool", o5, win[:, :, OK_:OK_ + 256].rearrange("p k (h d) -> p k h d", h=4), [b_win], [b_wkd])
            o5 = wqd[:].rearrange("p k (h r d) -> p k h r d", h=4, r=2)[:, :, :, r, :]
            kb.ts("pool", o5, win[:, :, OQ_:OQ_ + 256].rearrange("p k (h d) -> p k h d", h=4), 0.125, None, ALU.mult, None,
                  [b_win], [b_wqd])
        for d, (lrn, bn) in enumerate([("gla_lr_f", "gla_bias_f"), ("gla_lr_b", "gla_bias_b")]):
            t, b = lrd[d]
            for r in range(2):
                kb.dma(t[0:16, :].rearrange("k (h r d) -> k h r d", h=4, r=2)[:, :, r, :],
                       I[lrn][l].rearrange("k (h d) -> k h d", h=4), writes=[b])
                kb.dma(t[16:17, :].rearrange("k (h r d) -> k h r d", h=4, r=2)[:, :, r, :],
                       I[bn][l].rearrange("(o h d) -> o h d", o=1, h=4), writes=[b])
            kb.memset("dve", z1[d][0][:], 1.0, [z1[d][1]])

        groups = [(0, 2)] + [(2 + 4 * g, 4) for g in range(8)]
        for (i0, nt) in groups:
            Ng = 128 * nt
            isctx = i0 == 0
            Ai = 2 if isctx else 0
            need_out = (l == 0) or (2 <= i0 < 2 + NOWN // 128)
            need_q = need_out
            hT, b_hT = hTr.next()
            for j in range(nt):
                xt, b_xt = xtr.next()
                ss, b_ss = ssr.next()
                kb.dma(xt[:], S["LAT"][(i0 + j) * 128:(i0 + j + 1) * 128, :], reads=[db("LAT", i0 + j)], writes=[b_xt])
                kb.act(xs[:, j, :], xt[:], AF.Square, [b_xt], [b_xs, b_ss], scale=1.0 / 32.0, accum_out=ss[:, 0:1])
                kb.rstd(ss[:, 1:2], ss[:, 0:1], b_ss)
                kb.ts("dve", xs[:, j, :], xt[:], ss[:, 1:2], None, ALU.mult, None, [b_xt, b_ss], [b_xs])
            for k in range(KT):
                pt, b_pt = ptr.next()
                for j in range(nt):
                    kb.tr(pt[:, j * 128:(j + 1) * 128], xs[:, j, k * 128:(k + 1) * 128], identf[:], [b_xs, b_identf], [b_pt])
                if k % 2 == 0:
                    kb.ts("dve", hT[:, k, 0:Ng], pt[:, 0:Ng], ab[:, k, Ai:Ai + 1], ab[:, k, Ai + 1:Ai + 2], ALU.mult, ALU.add,
                          [b_pt, b_ab], [b_hT])
                else:
                    kb.act(hT[:, k, 0:Ng], pt[:, 0:Ng], AF.Identity, [b_pt, b_ab], [b_hT],
                           scale=ab[:, k, Ai:Ai + 1], bias=ab[:, k, Ai + 1:Ai + 2])

            kb.dump("xs", xs[:], [b_xs])
            kb.dump("hT", hT[:], [b_hT])
            kb.dump("win", win[:, 0, :], [b_win])
            def fm(W, bW, col0, M, perm=False):
                pf, b_pf = pfm.next()
                for k in range(KT):
                    rhs = hT[:, k, 0:Ng]
                    if perm:
                        rhs = rhs.rearrange("p (c a b) -> p b a c", c=4, a=8, b=8)
                    kb.mm(pf[0:M, 0:Ng], W[:, k, col0:col0 + M], rhs, k == 0, k == KT - 1, [bW, b_hT], [b_pf])
                return pf, b_pf

            ev = [0]

            def evac(out, in_, reads, writes):
                ev[0] += 1
                kb.cp("dve" if ev[0] % 2 else "act", out, in_, reads, writes)

            for ft in range(4):
                pf, b_pf = fm(win, b_win, OU_ + ft * 128, 128, perm=isctx)
                evac(ust[:, ft, 0:Ng], pf[:, 0:Ng], [b_pf], [b_ust])
            if isctx:
                kb.dma(S["UTC"].rearrange("(f p) t -> p f t", p=128), ust[:, :, 0:Ng], reads=[b_ust], writes=[db("UTC")])
            else:
                t0 = (i0 - 2) * 128
                kb.dma(S["UTL"].rearrange("(f p) t -> p f t", p=128)[:, :, t0:t0 + Ng], ust[:, :, 0:Ng], reads=[b_ust],
                       writes=[db("UTL")])
            if need_out:
                for nm, c0 in (("GAT", OGA), ("GMT", OGM)):
                    gst, b_gst = gstr.next()
                    for ft in range(8):
                        pf, b_pf = fm(win, b_win, c0 + ft * 128, 128)
                        kb.act(gst[:, ft, 0:Ng], pf[:, 0:Ng], AF.Sigmoid, [b_pf], [b_gst])
                    kb.dma(S[nm].rearrange("(f p) t -> p f t", p=128)[:, :, i0 * 128:i0 * 128 + Ng], gst[:, :, 0:Ng],
                           reads=[b_gst], writes=[db(nm)])
            for h in range(4):
                pf, b_pf = fm(wkd, b_wkd, h * 128, 128)
                evac(kq[:, h, 0:Ng], pf[:, 0:Ng], [b_pf], [b_kq])
            for h in range(4 if need_q else 0):
                pf, b_pf = fm(wqd, b_wqd, h * 128, 128)
                evac(kq[:, 4 + h, 0:Ng], pf[:, 0:Ng], [b_pf], [b_kq])
            for d, c0 in enumerate((OZF, OZB)):
                pf, b_pf = fm(win, b_win, c0, 16)
                evac(z1[d][0][0:16, 0:Ng], pf[0:16, 0:Ng], [b_pf], [z1[d][1]])

            for j in range(nt):
                i = i0 + j
                cs = slice(j * 128, (j + 1) * 128)

                def tm(W, bW, col0):
                    p_, b_ = ptm.next()
                    for k in range(KT):
                        kb.mm(p_[:, 0:512], hT[:, k, cs], W[:, k, col0:col0 + 512], k == 0, k == KT - 1, [bW, b_hT], [b_])
                    return p_, b_

                pv, b_pv = tm(win, b_win, OV_)
                vst, b_vst = vstr.next()
                evac(vst[:], pv[:], [b_pv], [b_vst])
                kb.dma(S["V"][i * 128:(i + 1) * 128, :], vst[:], reads=[b_vst], writes=[db("V")])
                if need_out:
                    pr, b_pr = tm(win, b_win, OR_)
                    rst, b_rst = rstr.next()
                    kb.act(rst[:], pr[:], AF.Silu, [b_pr], [b_rst])
                    kb.dma(S["GR"][i * 128:(i + 1) * 128, :], rst[:], reads=[b_rst], writes=[db("GR")])
                for d in range(2):
                    px, b_px = pg.next()
                    kb.mm(px[:, 0:512], z1[d][0][0:17, cs], lrd[d][0][0:17, :], True, True, [z1[d][1], lrd[d][1]], [b_px])
                    kb.act(etmp[:], px[:], AF.Exp, [b_px], [b_etmp], scale=-1.0)
                    kb.act(sp[d][0][:], etmp[:], AF.Ln, [b_etmp], [sp[d][1]], bias=1.0)
                pk, b_pk = tm(wkd, b_wkd, 0)
                kdst, b_kdst = kdstr.next()
                for d in range(2):
                    U, bU = (uf, b_uf) if d == 0 else (ub, b_ub)
                    pb_, b_pb = pg.next()
                    kb.mm(pb_[:, 0:512], U[:], sp[d][0][:], True, True, [bU, sp[d][1]], [b_pb])
                    kb.act(ektok[:], pb_[:], AF.Exp, [b_pb], [b_ektok], scale=-1.0)
                    kb.tt("dve", kdst[:, d, :], pk[:], ektok[:], ALU.mult, [b_pk, b_ektok], [b_kdst])
                kb.dma(S["KD"][i].rearrange("d p c -> p d c"), kdst[:], reads=[b_kdst], writes=[db("KD")])
                kqst, b_kqst = kqstr.next()
                for d in range(2):
                    U, bU = (uf, b_uf) if d == 0 else (ub, b_ub)
                    for h in range(4):
                        ph, b_ph = pg.next()
                        spd = sp[d][0][:, h * 128:(h + 1) * 128]
                        kb.mm(ph[:, 0:128], spd, U[:], True, True, [sp[d][1], bU], [b_ph])
                        if need_q:
                            kb.mm(ph[:, 128:256], spd, U[:], True, False, [sp[d][1], bU], [b_ph])
                            kb.mm(ph[:, 128:256], mba[0:2, :], mbb[0:2, :], False, True, [b_mba, b_mbb], [b_ph])
                        ek, b_ek = ekr.next()
                        eq, b_eq = eqr.next()
                        kb.act(ek[:], ph[:, 0:128], AF.Exp, [b_ph], [b_ek], scale=-1.0)
                        if need_q:
                            kb.act(eq[:], ph[:, 128:256], AF.Exp, [b_ph], [b_eq])
                        c0 = 63 if d == 0 else 0
                        kb.act(dec[:, i, d, :, h], ph[:, c0:128:64], AF.Exp, [b_ph], [b_dec])
                        kb.tt("dve", kqst[:, d, h, 0, :], kq[:, h, cs], ek[:], ALU.mult, [b_kq, b_ek], [b_kqst])
                        if need_q:
                            kb.tt("pool", kqst[:, d, h, 1, :], kq[:, 4 + h, cs], eq[:], ALU.mult, [b_kq, b_eq], [b_kqst])
                kb.dma(S["KQT"][i], kqst[:], reads=[b_kqst], writes=[db("KQT")])
        kb.P.barrier()


def phase_g(kb, I, S, l, C):
    db = kb.db
    identb, b_identb = C["identb"]
    amf, b_amf = C["amf"]
    amb, b_amb = C["amb"]
    dec, b_dec = C["dec"]
    gnw, b_gnw = C["gnw"]
    am = [(amf, b_amf), (amb, b_amb)]
    with ExitStack() as es:
        sstk = [kb.sb(es, "sstk%d" % d, [128, NTILE, 4, 128], BF16) for d in range(2)]
        srun = [kb.sb(es, "srun%d" % d, [128, 4, 128], F32) for d in range(2)]
        tmpr = kb.sbr(es, "tmpS", [128, 4, 128], F32, 2)
        kdr = kb.sbr(es, "kdt", [128, 512], BF16, 3)
        vr = kb.sbr(es, "vt", [128, 512], BF16, 3)
        kqr = kb.sbr(es, "kqt", [128, 2, 4, 2, 128], BF16, 2)
        grr = kb.sbr(es, "grt", [128, 512], BF16, 2)
        attr = kb.sbr(es, "attm", [128, 128], BF16, 4)
        junk, b_junk = kb.sb(es, "junkg", [128, 128], F32)
        ssqr = kb.sbr(es, "ssq", [128, 8], F32, 2)
        onr = kb.sbr(es, "on", [128, 4, 128], F32, 2)
        ogr = kb.sbr(es, "og", [128, 512], BF16, 2)
        ogTr = kb.sbr(es, "ogT", [128, 4, 128], BF16, 2)
        pP = kb.psr(es, "pP", [128, 512], 2)
        patt = kb.psr(es, "patt", [128, 512], 2)
        po_r = kb.psr(es, "po", [128, 512], 2)
        ptb = kb.psr(es, "ptb", [128, 4, 128], 2, dt=BF16)

        for d in range(2):
            kb.memset("dve", srun[d][0][:], 0.0, [srun[d][1]])
        own_hi = 2 + NOWN // 128
        orders = [list(range(own_hi if l == 1 else NTILE)), [1, 0] + list(range(NTILE - 1, 1, -1))]
        for step in range(NTILE):
            for d in range(2):
                if step >= len(orders[d]):
                    continue
                i = orders[d][step]
                kd, b_kd = kdr.next()
                v, b_v = vr.next()
                kb.dma(kd[:], S["KD"][i, d], reads=[db("KD")], writes=[b_kd])
                kb.dma(v[:], S["V"][i * 128:(i + 1) * 128, :], reads=[db("V")], writes=[b_v])
                sr, b_sr = srun[d]
                st, b_st = sstk[d]
                for c in ((0, 1) if d == 0 else (1, 0)):
                    hs = slice(c * 64, (c + 1) * 64)
                    kb.cp("act", st[hs, i, :, :], sr[hs, :, :], [b_sr], [b_st])
                    pp, b_pp = pP.next()
                    for h in range(4):
                        fs = slice(h * 128, (h + 1) * 128)
                        kb.mm(pp[:, fs], kd[hs, fs], v[hs, fs], True, True, [b_kd, b_v], [b_pp])
                    tmp, b_tmp = tmpr.next()
                    kb.tt("dve", tmp[:], pp[:].rearrange("p (h e) -> p h e", h=4), sr[:], ALU.add, [b_pp, b_sr], [b_tmp])
                    kb.tt("dve", sr[:], tmp[:], dec[:, i, d, c, :].unsqueeze(2).to_broadcast([128, 4, 128]), ALU.mult,
                          [b_tmp, b_dec], [b_sr])
        kb.dump("srun0", srun[0][0][:], [srun[0][1]])
        kb.dump("sstk0", sstk[0][0][:], [sstk[0][1]])

        for i in range(NTILE):
            if l == 1 and (i < 2 or i >= own_hi):
                continue
            kqt, b_kqt = kqr.next()
            v, b_v = vr.next()
            gr, b_gr = grr.next()
            kb.dma(kqt[:], S["KQT"][i], reads=[db("KQT")], writes=[b_kqt])
            kb.dma(v[:], S["V"][i * 128:(i + 1) * 128, :], reads=[db("V")], writes=[b_v])
            kb.dma(gr[:], S["GR"][i * 128:(i + 1) * 128, :], reads=[db("GR")], writes=[b_gr])
            po, b_po = po_r.next()
            for h in range(4):
                fs = slice(h * 128, (h + 1) * 128)
                atts = []
                for d in range(2):
                    slot = (h * 2 + d) % 4
                    if slot == 0:
                        pa, b_pa = patt.next()
                    kb.mm(pa[:, slot * 128:(slot + 1) * 128], kqt[:, d, h, 0, :], kqt[:, d, h, 1, :], True, True,
                          [b_kqt], [b_pa])
                    at, b_at = attr.next()
                    kb.tt("dve", at[:], pa[:, slot * 128:(slot + 1) * 128], am[d][0][:], ALU.mult, [b_pa, am[d][1]], [b_at])
                    atts.append((at, b_at))
                kb.mm(po[:, fs], atts[0][0][:], v[:, fs], True, False, [atts[0][1], b_v], [b_po])
                kb.mm(po[:, fs], kqt[:, 0, h, 1, :], sstk[0][0][:, i, h, :], False, False, [b_kqt, sstk[0][1]], [b_po])
                kb.mm(po[:, fs], atts[1][0][:], v[:, fs], False, False, [atts[1][1], b_v], [b_po])
                kb.mm(po[:, fs], kqt[:, 1, h, 1, :], sstk[1][0][:, i, h, :], False, True, [b_kqt, sstk[1][1]], [b_po])
            ssq, b_ssq = ssqr.next()
            for h in range(4):
                kb.act(junk[:], po[:, h * 128:(h + 1) * 128], AF.Square, [b_po], [b_junk, b_ssq],
                       scale=1.0 / math.sqrt(128.0), accum_out=ssq[:, h:h + 1])
            kb.act(ssq[:, 4:8], ssq[:, 0:4], AF.Sqrt, [b_ssq], [b_ssq], bias=EPS, scale=1.0)
            kb.P.op("dve", lambda E, ssq=ssq: E.reciprocal(out=ssq[:, 4:8], in_=ssq[:, 4:8]), [b_ssq], [b_ssq])
            on, b_on = onr.next()
            kb.tt("dve", on[:], po[:].rearrange("p (h e) -> p h e", h=4),
                  ssq[:, 4:8].unsqueeze(2).to_broadcast([128, 4, 128]), ALU.mult, [b_po, b_ssq], [b_on])
            og, b_og = ogr.next()
            kb.tt("pool", og[:], on[:].rearrange("p h e -> p (h e)"), gr[:], ALU.mult, [b_on, b_gr], [b_og])
            pt, b_pt = ptb.next()
            for h in range(4):
                kb.tr(pt[:, h, :], og[:, h * 128:(h + 1) * 128], identb[:], [b_og, b_identb], [b_pt])
            ogT, b_ogT = ogTr.next()
            kb.act(ogT[:], pt[:], AF.Copy, [b_pt, b_gnw], [b_ogT], scale=gnw[:, 0:1])
            kb.dma(S["OGT"].rearrange("(h e) t -> e h t", h=4)[:, :, i * 128:(i + 1) * 128], ogT[:], reads=[b_ogT],
                   writes=[db("OGT")])
        kb.P.barrier()


MAGIC = 12582912.0
TWO_PI = 2.0 * math.pi
COL_ORDER = [list(range(68)), [3, 2, 1, 0] + list(range(67, 3, -1))]


def phase_s5(kb, I, S, l, C, last=False):
    db = kb.db
    nc = kb.nc
    identf, b_identf = C["identf"]
    identb, b_identb = C["identb"]
    with ExitStack() as es:
        aT, b_aT = kb.sb(es, "aT", [64, 4, 32], F32)
        cT, b_cT = kb.sb(es, "cT", [64, 2, 512], F32)
        dtt, b_dt = kb.sb(es, "dtt", [64, 2, 32], F32)
        ardt, b_ardt = kb.sb(es, "ardt", [64, 2, 32], F32)
        aidt, b_aidt = kb.sb(es, "aidt", [64, 2, 32], F32)
        Bri, b_Bri = kb.sb(es, "Bri", [64, 2, 512], F32)
        Bb, b_Bb = kb.sb(es, "Bb", [64, 2, 2, 512], F32)
        nv, b_nv = kb.sb(es, "nv", [64, 2, NVEC], F32)
        dcol, b_dcol = kb.sb(es, "dcol", [128, 32], F32)
        HBbd = [kb.sb(es, "HBb%d" % d, [64, 2, 32, 68], BF16) for d in range(2)]
        yctx, b_yctx = kb.sb(es, "yctx", [128, 32, 8, 4], F32)
        ur = kb.sbr(es, "ug", [128, 8, 68], BF16, 3)
        mge, b_mge = kb.sb(es, "mge", [128, 128], F32)
        mle, b_mle = kb.sb(es, "mle", [128, 128], F32)

        def load_u(g):
            u, b_u = ur.next()
            kb.ugid = getattr(kb, "ugid", 0) + 1
            for sl in range(8):
                src = bass.AP(tensor=S["UTL"].tensor, offset=g * 16 * NLAT + sl * 64, ap=[[NLAT, 16], [512, 8], [1, 64]])
                kb.dma(u[sl * 16:(sl + 1) * 16, :, 4:68], src, reads=[db("UTL")], writes=[b_u], gid=("u", kb.ugid))
                src = bass.AP(tensor=S["UTC"].tensor, offset=g * 16 * NCTX + sl * 32, ap=[[NCTX, 16], [4, 8], [1, 4]])
                kb.dma(u[sl * 16:(sl + 1) * 16, :, 0:4], src, reads=[db("UTC")], writes=[b_u], gid=("u", kb.ugid))
            return u, b_u

        with ExitStack() as es1:
            rows, b_rows = kb.sb(es1, "rows", [128, 9, 64], F32)
            ptr, b_ptr = kb.ps(es1, "ptrs", [64, 512])
            for w_, nm in enumerate(["s5_a_re_f", "s5_a_im_f", "s5_a_re_b", "s5_a_im_b"]):
                kb.dma(rows[w_ * 32:(w_ + 1) * 32, 0, :], I[nm][l], writes=[b_rows])
            for ri, nm in enumerate(["s5_c_re", "s5_c_im"]):
                kb.dma(rows[:, 1 + 4 * ri:5 + 4 * ri, :], I[nm][l].rearrange("(t g) h p -> (g h) t p", t=4), writes=[b_rows])
            kb.tr(ptr[:, 0:128], rows[:, 0, :], identf[:], [b_rows, b_identf], [b_ptr])
            kb.cp("dve", aT[:].rearrange("p w g -> p (w g)"), ptr[:, 0:128], [b_ptr], [b_aT])
            for ri in range(2):
                for t in range(4):
                    kb.tr(ptr[:, t * 128:(t + 1) * 128], rows[:, 1 + 4 * ri + t, :], identf[:], [b_rows, b_identf], [b_ptr])
                kb.cp("dve", cT[:, ri, :], ptr[:, 0:512], [b_ptr], [b_cT])
            for d, nm in enumerate(["s5_log_dt_f", "s5_log_dt_b"]):
                src = bass.AP(tensor=I[nm].tensor, offset=l * 32, ap=[[0, 64], [1, 32]])
                kb.dma(dtt[:, d, :], src, writes=[b_dt])
            kb.act(dtt[:], dtt[:], AF.Exp, [b_dt], [b_dt])
            for d in range(2):
                kb.tt("dve", ardt[:, d, :], aT[:, 2 * d, :], dtt[:, d, :], ALU.mult, [b_aT, b_dt], [b_ardt])
                kb.tt("dve", aidt[:, d, :], aT[:, 2 * d + 1, :], dtt[:, d, :], ALU.mult, [b_aT, b_dt], [b_aidt])
            kb.dma(Bri[:, 0, :].rearrange("p (g h) -> p g h", h=16), I["s5_b_re"][l].rearrange("g p h -> p g h"), writes=[b_Bri])
            kb.dma(Bri[:, 1, :].rearrange("p (g h) -> p g h", h=16), I["s5_b_im"][l].rearrange("g p h -> p g h"), writes=[b_Bri])
            kb.dma(nv[:].rearrange("p d n -> p (d n)"),
                   bass.AP(tensor=I["k_nvec"].tensor, offset=0, ap=[[0, 64], [1, 2 * NVEC]]), writes=[b_nv])
            for sl in range(8):
                kb.dma(dcol[sl * 16:(sl + 1) * 16, :], I["s5_d"][l].rearrange("(g h) -> h g", h=16), writes=[b_dcol],
                       gid=("dcol", l), allow_slow_non_contiguous=True)
            kb.dma(mge[:], I["k_mge"], writes=[b_mge])
            kb.dma(mle[:], I["k_mle"], writes=[b_mle])
            kb.P.barrier()

        def pw_tables(es_, lo, hi, tag):
            n = hi - lo
            PW, b_PW = kb.sb(es_, "PW" + tag, [64, 2, 2, 32, n], F32)
            with ExitStack() as e2:
                E_, b_E = kb.sb(e2, "pwE", [64, 32, n], F32)
                th, b_th = kb.sb(e2, "pwT", [64, 32, n], F32)
                ar, b_ar = kb.sb(e2, "pwA", [64, 32, n], F32)
                kk, b_kk = kb.sb(e2, "pwK", [64, 32, n], F32)
                for d in range(2):
                    nvb = nv[:, d, lo:hi].unsqueeze(1).to_broadcast([64, 32, n])
                    kb.tt("dve", E_[:], ardt[:, d, :].unsqueeze(2).to_broadcast([64, 32, n]), nvb, ALU.mult,
                          [b_ardt, b_nv], [b_E])
                    kb.act(E_[:], E_[:], AF.Exp, [b_E], [b_E])
                    kb.tt("dve", th[:], aidt[:, d, :].unsqueeze(2).to_broadcast([64, 32, n]), nvb, ALU.mult,
                          [b_aidt, b_nv], [b_th])
                    for ri, ph in ((0, 0.5 * math.pi), (1, 0.0)):
                        kb.ts("dve", ar[:], th[:], ph, None, ALU.add, None, [b_th], [b_ar])
                        kb.ts("dve", kk[:], ar[:], 1.0 / TWO_PI, MAGIC, ALU.mult, ALU.add, [b_ar], [b_kk])
                        kb.ts("dve", kk[:], kk[:], -MAGIC, None, ALU.add, None, [b_kk], [b_kk])
                        kb.stt("dve", ar[:], kk[:], -TWO_PI, ar[:], ALU.mult, ALU.add, [b_kk, b_ar], [b_ar])
                        kb.act(ar[:], ar[:], AF.Sin, [b_ar], [b_ar])
                        kb.tt("dve", PW[:, d, ri, :, :], ar[:], E_[:], ALU.mult, [b_ar, b_E], [b_PW])
                kb.P.barrier()
            return PW, b_PW

        def cmul(eng, out_re, out_im, a_re, a_im, b_re, b_im, t1, t2, reads, w_re, w_im, b_t, neg_im=False):
            kb.tt(eng, t1, a_re, b_re, ALU.mult, reads, [b_t])
            kb.tt(eng, t2, a_im, b_im, ALU.mult, reads, [b_t])
            kb.tt(eng, out_re, t1, t2, ALU.subtract, [b_t], [w_re])
            kb.tt(eng, t1, a_re, b_im, ALU.mult, reads, [b_t])
            kb.tt(eng, t2, a_im, b_re, ALU.mult, reads, [b_t])
            if neg_im:
                kb.tt(eng, t1, t1, t2, ALU.add, [b_t], [b_t])
                kb.ts(eng, out_im, t1, -1.0, None, ALU.mult, None, [b_t], [w_im])
            else:
                kb.tt(eng, out_im, t1, t2, ALU.add, [b_t], [w_im])

        s5stop = getattr(kb, "s5stop", 9)
        if s5stop <= 1:
            kb.dump("aT", aT[:], [b_aT]); kb.dump("cT", cT[:], [b_cT]); kb.dump("dtt", dtt[:], [b_dt]); kb.dump("nv", nv[:], [b_nv])
            kb.dump("dcol", dcol[:], [b_dcol]); kb.dump("Bri", Bri[:], [b_Bri])
            kb.P.barrier()
            return
        with ExitStack() as es1:
            PWq = []
            for d, idx in ((0, 72 + 62), (1, 72 + 1)):
                PWq.append(pw_tables(es1, idx, idx + 1, "q%d" % d))
            q, b_q = kb.sb(es1, "q", [64, 8, 32], F32)
            t1, b_t = kb.sb(es1, "qt", [64, 2, 512], F32)
            for d in range(2):
                PWd, b_PWd = PWq[d]
                lbr = PWd[:, d, 0, :, 0]
                lbi = PWd[:, d, 1, :, 0]
                are, aim = aT[:, 2 * d, :], aT[:, 2 * d + 1, :]
                kb.ts("dve", q[:, 0, :], lbr, -1.0, None, ALU.add, None, [b_PWd], [b_q])
                kb.tt("dve", q[:, 1, :], are, are, ALU.mult, [b_aT], [b_q])
                kb.tt("dve", q[:, 2, :], aim, aim, ALU.mult, [b_aT], [b_q])
                kb.tt("dve", q[:, 1, :], q[:, 1, :], q[:, 2, :], ALU.add, [b_q], [b_q])
                kb.P.op("dve", lambda E, q=q: E.reciprocal(out=q[:, 1, :], in_=q[:, 1, :]), [b_q], [b_q])
                kb.tt("dve", q[:, 2, :], q[:, 0, :], are, ALU.mult, [b_q, b_aT], [b_q])
                kb.tt("dve", q[:, 3, :], lbi, aim, ALU.mult, [b_PWd, b_aT], [b_q])
                kb.tt("dve", q[:, 2, :], q[:, 2, :], q[:, 3, :], ALU.add, [b_q], [b_q])
                kb.tt("dve", q[:, 4, :], q[:, 2, :], q[:, 1, :], ALU.mult, [b_q], [b_q])
                kb.tt("dve", q[:, 2, :], lbi, are, ALU.mult, [b_PWd, b_aT], [b_q])
                kb.tt("dve", q[:, 3, :], q[:, 0, :], aim, ALU.mult, [b_q, b_aT], [b_q])
                kb.tt("dve", q[:, 2, :], q[:, 2, :], q[:, 3, :], ALU.subtract, [b_q], [b_q])
                kb.tt("dve", q[:, 5, :], q[:, 2, :], q[:, 1, :], ALU.mult, [b_q], [b_q])
                qre = q[:, 4, :].unsqueeze(2).to_broadcast([64, 32, 16])
                qim = q[:, 5, :].unsqueeze(2).to_broadcast([64, 32, 16])
                v3 = lambda ap: ap.rearrange("p (g h) -> p g h", h=16)
                cmul("dve", v3(Bb[:, d, 0, :]), v3(Bb[:, d, 1, :]), qre, qim, v3(Bri[:, 0, :]), v3(Bri[:, 1, :]),
                     v3(t1[:, 0, :]), v3(t1[:, 1, :]), [b_q, b_Bri], b_Bb, b_Bb, b_t)
            kb.P.barrier()

        if s5stop <= 2:
            kb.dump("Bb", Bb[:], [b_Bb])
            kb.P.barrier()
            return
        with ExitStack() as es1:
            PW1, b_PW1 = pw_tables(es1, 72, 137, "1")
            HL, b_HL = kb.sb(es1, "HL", [64, 2, 2, 32, 68], F32)
            with ExitStack() as es2:
                Xr = [kb.sbr(es2, "X%d" % d, [64, 2, 1024], BF16, 2) for d in range(2)]
                tr_ = [kb.sbr(es2, "xt12%d" % d, [64, 2, 1024], F32, 2) for d in range(2)]
                SIr = kb.sbr(es2, "SI", [128, 2, 2, 8, 64], BF16, 2)
                ptx = kb.psr(es2, "ptx", [128, 2048], 2, dt=BF16)
                phl = kb.psr(es2, "phl", [64, 4, 128], 2)
                def gen1(g):
                    Xd = [Xr[d].next() for d in range(2)]
                    for d in range(2):
                        X, b_X = Xd[d]
                        t12, b_t12 = tr_[d].next()
                        v3 = lambda ap: ap.rearrange("p (s h) -> p s h", h=16)
                        pre = PW1[:, d, 0, g, 0:64].unsqueeze(2).to_broadcast([64, 64, 16])
                        pim = PW1[:, d, 1, g, 0:64].unsqueeze(2).to_broadcast([64, 64, 16])
                        bre = Bb[:, d, 0, g * 16:(g + 1) * 16].unsqueeze(1).to_broadcast([64, 64, 16])
                        bim = Bb[:, d, 1, g * 16:(g + 1) * 16].unsqueeze(1).to_broadcast([64, 64, 16])
                        cmul("dve", v3(X[:, 0, :]), v3(X[:, 1, :]), pre, pim, bre, bim, v3(t12[:, 0, :]), v3(t12[:, 1, :]),
                             [b_PW1, b_Bb], b_X, b_X, b_t12)
                    return Xd

                def use1(g, Xd):
                    u, b_u = load_u(g)
                    px, b_px = ptx.next()
                    SI, b_SI = SIr.next()
                    for d in range(2):
                        for ri in range(2):
                            for sh in range(8):
                                o_ = ((d * 2 + ri) * 8 + sh) * 64
                                kb.tr(px[:, o_:o_ + 64], Xd[d][0][:, ri, sh * 128:(sh + 1) * 128], identb[0:64, 0:64],
                                      [Xd[d][1], b_identb], [b_px])
                    for q4 in range(4):
                        kb.cp("act", SI[:].rearrange("p d r s c -> p (d r s c)")[:, q4 * 512:(q4 + 1) * 512],
                              px[:, q4 * 512:(q4 + 1) * 512], [b_px], [b_SI])
                    ph, b_ph = phl.next()
                    for d in range(2):
                        for ri in range(2):
                            for sh in range(8):
                                kb.mm(ph[:, d * 2 + ri, 0:68], SI[:, d, ri, sh, :], u[:, sh, :], sh == 0, sh == 7,
                                      [b_SI, b_u], [b_ph])
                    kb.cp("act", HL[:, :, :, g, :].rearrange("p d r c -> p (d r) c"), ph[:, :, 0:68], [b_ph], [b_HL])

                Xn = gen1(0)
                for g in range(32):
                    Xc = Xn
                    if g + 1 < 32:
                        Xn = gen1(g + 1)
                    use1(g, Xc)
                kb.P.barrier()
            if s5stop <= 3:
                kb.dump("HL", HL[:], [b_HL])
                kb.P.barrier()
                return
            with ExitStack() as es2:
                st = [kb.sb(es2, "sct%d" % d, [64, 4, 32], F32) for d in range(2)]
                sc = [kb.sb(es2, "scs%d" % d, [64, 2, 2, 32], F32) for d in range(2)]
                for d in range(2):
                    eng = "dve" if d == 0 else "pool"
                    kb.memset(eng, sc[d][0][:], 0.0, [sc[d][1]])
                    kb.memset(eng, HBbd[d][0][:, :, :, COL_ORDER[d][0]], 0.0, [HBbd[d][1]])
                for j in range(67):
                    for d in range(2):
                        eng = "dve" if d == 0 else "pool"
                        t, b_t = st[d]
                        s_, b_s = sc[d]
                        cur, nxt = COL_ORDER[d][j], COL_ORDER[d][j + 1]
                        pi, po = j % 2, (j + 1) % 2
                        lre, lim = PW1[:, d, 0, :, 64], PW1[:, d, 1, :, 64]
                        hre, him = s_[:, pi, 0, :], s_[:, pi, 1, :]
                        kb.tt(eng, t[:, 0, :], lre, hre, ALU.mult, [b_PW1, b_s], [b_t])
                        kb.tt(eng, t[:, 1, :], lim, him, ALU.mult, [b_PW1, b_s], [b_t])
                        kb.tt(eng, t[:, 0, :], t[:, 0, :], t[:, 1, :], ALU.subtract, [b_t], [b_t])
                        kb.tt(eng, s_[:, po, 0, :], t[:, 0, :], HL[:, d, 0, :, cur], ALU.add, [b_t, b_HL], [b_s])
                        kb.tt(eng, t[:, 2, :], lre, him, ALU.mult, [b_PW1, b_s], [b_t])
                        kb.tt(eng, t[:, 3, :], lim, hre, ALU.mult, [b_PW1, b_s], [b_t])
                        kb.tt(eng, t[:, 2, :], t[:, 2, :], t[:, 3, :], ALU.add, [b_t], [b_t])
                        kb.tt(eng, s_[:, po, 1, :], t[:, 2, :], HL[:, d, 1, :, cur], ALU.add, [b_t, b_HL], [b_s])
                        kb.cp(eng, HBbd[d][0][:, 0, :, nxt], s_[:, po, 0, :], [b_s], [HBbd[d][1]])
                        kb.ts(eng, HBbd[d][0][:, 1, :, nxt], s_[:, po, 1, :], -1.0, None, ALU.mult, None, [b_s], [HBbd[d][1]])
                kb.P.barrier()

        if s5stop <= 4:
            kb.P.barrier()
            return
        with ExitStack() as es1:
            PW2, b_PW2 = pw_tables(es1, 0, 72, "2")
            with ExitStack() as es2:
                CLr = [kb.sbr(es2, "CL%d" % d, [64, 2, 1024], BF16, 2) for d in range(2)]
                RSr = [kb.sbr(es2, "RS%d" % d, [64, 2, 128], BF16, 2) for d in range(2)]
                t2r = [kb.sbr(es2, "t2%d" % d, [64, 2, 1024], F32, 2) for d in range(2)]
                TBr = kb.sbr(es2, "TB", [128, 2, 8, 128], BF16, 2)
                t0r = kb.sbr(es2, "t0", [128, 2, 128], F32, 2)
                ysr = kb.sbr(es2, "ys", [128, 8, 68], F32, 2)
                pT = kb.psr(es2, "pT", [128, 2048], 1)
                pY = kb.psr(es2, "pY", [128, 8, 128], 2)
                NTH = 4 if last else 8

                def gen2(g):
                    CLd = [CLr[d].next() for d in range(2)]
                    RSd = [RSr[d].next() for d in range(2)]
                    for d in range(2):
                        CL, b_CL = CLd[d]
                        RS, b_RS = RSd[d]
                        t2, b_t2 = t2r[d].next()
                        v3 = lambda ap: ap.rearrange("p (s h) -> p s h", h=16)
                        pre = PW2[:, d, 0, g, 0:64].unsqueeze(2).to_broadcast([64, 64, 16])
                        pim = PW2[:, d, 1, g, 0:64].unsqueeze(2).to_broadcast([64, 64, 16])
                        cre = cT[:, 0, g * 16:(g + 1) * 16].unsqueeze(1).to_broadcast([64, 64, 16])
                        cim = cT[:, 1, g * 16:(g + 1) * 16].unsqueeze(1).to_broadcast([64, 64, 16])
                        cmul("dve", v3(CL[:, 0, :]), v3(CL[:, 1, :]), pre, pim, cre, cim, v3(t2[:, 0, :]), v3(t2[:, 1, :]),
                             [b_PW2, b_cT], b_CL, b_CL, b_t2)
                        pre = PW2[:, d, 0, g, 64:72].unsqueeze(2).to_broadcast([64, 8, 16])
                        pim = PW2[:, d, 1, g, 64:72].unsqueeze(2).to_broadcast([64, 8, 16])
                        bre = Bb[:, d, 0, g * 16:(g + 1) * 16].unsqueeze(1).to_broadcast([64, 8, 16])
                        bim = Bb[:, d, 1, g * 16:(g + 1) * 16].unsqueeze(1).to_broadcast([64, 8, 16])
                        cmul("dve", v3(RS[:, 0, :]), v3(RS[:, 1, :]), pre, pim, bre, bim, v3(t2[:, 0, 0:128]),
                             v3(t2[:, 1, 0:128]), [b_PW2, b_Bb], b_RS, b_RS, b_t2, neg_im=True)
                    return CLd, RSd

                def use2(g, CLd, RSd):
                    u, b_u = load_u(g)
                    pt, b_pt = pT.next()
                    for d in range(2):
                        for dl in range(8):
                            o_ = (d * 8 + dl) * 128
                            kb.mm(pt[:, o_:o_ + 128], RSd[d][0][:, 0, :], CLd[d][0][:, 0, dl * 128:(dl + 1) * 128], True, False,
                                  [RSd[d][1], CLd[d][1]], [b_pt])
                            kb.mm(pt[:, o_:o_ + 128], RSd[d][0][:, 1, :], CLd[d][0][:, 1, dl * 128:(dl + 1) * 128], False, True,
                                  [RSd[d][1], CLd[d][1]], [b_pt])
                    TB, b_TB = TBr.next()
                    for q4 in range(4):
                        kb.cp("act", TB[:].rearrange("p d s c -> p (d s c)")[:, q4 * 512:(q4 + 1) * 512],
                              pt[:, q4 * 512:(q4 + 1) * 512], [b_pt], [b_TB])
                    t0, b_t0 = t0r.next()
                    kb.tt("dve", t0[:, 0, :], pt[:, 0:128], mge[:], ALU.mult, [b_pt, b_mge, b_TB], [b_t0])
                    kb.tt("dve", t0[:, 1, :], pt[:, 1024:1152], mle[:], ALU.mult, [b_pt, b_mle, b_TB], [b_t0])
                    kb.tt("dve", t0[:, 0, :], t0[:, 0, :], t0[:, 1, :], ALU.add, [b_t0], [b_t0])
                    kb.stt("dve", TB[:, 0, 0, :], identf[:], dcol[:, g:g + 1], t0[:, 0, :], ALU.mult, ALU.add,
                           [b_identf, b_dcol, b_t0], [b_TB])
                    py, b_py = pY.next()
                    for th_ in range(NTH):
                        for sh in range(8):
                            dl = th_ - sh
                            lhsT = TB[:, 0, dl, :] if dl >= 0 else TB[:, 1, -dl, :]
                            kb.mm(py[:, th_, 0:68], lhsT, u[:, sh, :], sh == 0, False, [b_TB, b_u], [b_py])
                        jf = th_ * 128
                        jb = (7 - th_) * 128
                        kb.mm(py[:, th_, 0:68], CLd[0][0][:, 0, jf:jf + 128], HBbd[0][0][:, 0, g, :], False, False,
                              [CLd[0][1], HBbd[0][1]], [b_py])
                        kb.mm(py[:, th_, 0:68], CLd[0][0][:, 1, jf:jf + 128], HBbd[0][0][:, 1, g, :], False, False,
                              [CLd[0][1], HBbd[0][1]], [b_py])
                        kb.mm(py[:, th_, 0:68], CLd[1][0][:, 0, jb:jb + 128], HBbd[1][0][:, 0, g, :], False, False,
                              [CLd[1][1], HBbd[1][1]], [b_py])
                        kb.mm(py[:, th_, 0:68], CLd[1][0][:, 1, jb:jb + 128], HBbd[1][0][:, 1, g, :], False, True,
                              [CLd[1][1], HBbd[1][1]], [b_py])
                    ys, b_ys = ysr.next()
                    kb.cp("act", ys[:, 0:4, :], py[:, 0:4, 0:68], [b_py], [b_ys])
                    if not last:
                        kb.cp("act", ys[:, 4:8, :], py[:, 4:8, 0:68], [b_py], [b_ys])
                    for tl in range(8):
                        dst = bass.AP(tensor=S["YTL"].tensor, offset=g * 16 * NLAT + tl * 64, ap=[[NLAT, 16], [512, NTH], [1, 64]])
                        kb.dma(dst, ys[tl * 16:(tl + 1) * 16, 0:NTH, 4:68], reads=[b_ys], writes=[db("YTL")])
                    if not last:
                        kb.cp("pool", yctx[:, g, :, :], ys[:, :, 0:4], [b_ys], [b_yctx])

                gn = gen2(0)
                for g in range(32):
                    gc = gn
                    if g + 1 < 32:
                        gn = gen2(g + 1)
                    use2(g, *gc)
                for tl in range(8 if (s5stop > 6 and not last) else 0):
                    dst = bass.AP(tensor=S["YTC"].tensor, offset=tl * 32, ap=[[NCTX, 16], [16 * NCTX, 32], [1, 32]])
                    kb.dma(dst, yctx[tl * 16:(tl + 1) * 16, :, :, :].rearrange("p g a c -> p g (a c)"), reads=[b_yctx],
                           writes=[db("YTC")])
                kb.P.barrier()
            kb.P.barrier()
        kb.P.barrier()


def phase_m(kb, I, S, l, C, last):
    db = kb.db
    identf, b_identf = C["identf"]
    vpt, b_vpt = C["vpt"]
    ab, b_ab = C["ab"]
    gbc, b_gbc = C["gbc"]
    gates, b_gates = C["gates"]
    with ExitStack() as es:
        wgp, b_wgp = kb.sb(es, "wgp", [128, 4, D], BF16)
        wglu, b_wglu = kb.sb(es, "wglu", [128, 4, 512], BF16)
        ws5, b_ws5 = kb.sb(es, "ws5", [128, 4, D], BF16)
        wout, b_wout = kb.sb(es, "wout", [128, KT, D], BF16)
        kb.dma(wgp[:], I["w_gla_proj"][l].rearrange("(k p) n -> p k n", p=128), writes=[b_wgp], q="pool")
        kb.dma(wglu[:], I["s5_w_glu"][l].rearrange("(k p) n -> p k n", p=128), writes=[b_wglu], q="pool")
        kb.dma(ws5[:], I["w_s5_proj"][l].rearrange("(k p) n -> p k n", p=128), writes=[b_ws5], q="pool")
        for k in range(KT):
            kb.dma(wout[:, k, :], I["w_out"][l][k * 128:(k + 1) * 128, :], writes=[b_wout], q="pool", gid=("wout", l))
        if last:
            wr, b_wr = kb.sb(es, "wr", [128, KT, NEXP], F32)
            kb.dma(wr[:], I["moe_router"][0].rearrange("(k p) e -> p k e", p=128), writes=[b_wr])
        ogr = kb.sbr(es, "ogt", [128, 4, 512], BF16, 1)
        ytr = kb.sbr(es, "yt", [128, 4, 512], F32, 1)
        gar = kb.sbr(es, "gat", [128, 8, 512], BF16, 1)
        gmr = kb.sbr(es, "gmt", [128, 8, 512], BF16, 1)
        gt, b_gt = kb.sb(es, "gelt", [128, 4, 512], F32)
        sgm, b_sgm = kb.sb(es, "gelsg", [128, 4, 512], F32)
        s1, b_s1 = kb.sb(es, "s1", [128, 4, 512], BF16)
        s2, b_s2 = kb.sb(es, "s2", [128, 4, 512], BF16)
        sigr = kb.sbr(es, "sig", [128, 512], F32, 2)
        mT, b_mT = kb.sb(es, "mT", [128, 8, 512], BF16)
        t1r = kb.sbr(es, "t1", [128, 512], F32, 2)
        t2r = kb.sbr(es, "t2m", [128, 512], F32, 2)
        ltr = kb.sbr(es, "lt", [128, D], F32, 2)
        tmpr = kb.sbr(es, "tmpm", [128, D], F32, 1)
        l2r = kb.sbr(es, "lat2", [128, D], F32, 2)
        xs2r = kb.sbr(es, "xs2", [128, D], F32, 1)
        ssr = kb.sbr(es, "ssm", [128, 2], F32, 4)
        h2fr = kb.sbr(es, "h2f", [128, KT, 128], F32, 2)
        h2br = kb.sbr(es, "h2b", [128, KT, 128], BF16, 2)
        lgr = kb.sbr(es, "lg", [128, 8, 8], F32, 2)
        pab = kb.psr(es, "pab", [128, 512], 3)
        pml = kb.psr(es, "pml", [128, D], 1)
        pt2 = kb.psr(es, "pt2", [128, KT, 128], 1)
        plg = kb.psr(es, "plg", [128, 8], 1)

        if last:
            groups = [(2 + 4 * g, 4) for g in range(NOWN // 512)]
        else:
            groups = [(0, 2)] + [(2 + 4 * g, 4) for g in range(8)]
        for (i0, nt) in groups:
            Ng = 128 * nt
            isctx = i0 == 0
            T0 = i0 * 128
            og, b_og = ogr.next()
            yt, b_yt = ytr.next()
            ga, b_ga = gar.next()
            gm, b_gm = gmr.next()
            kb.dma(og[:, :, 0:Ng], S["OGT"].rearrange("(k p) t -> p k t", p=128)[:, :, T0:T0 + Ng], reads=[db("OGT")], writes=[b_og])
            if isctx:
                kb.dma(yt[:, :, 0:Ng], S["YTC"].rearrange("(k p) t -> p k t", p=128), reads=[db("YTC")], writes=[b_yt])
            else:
                kb.dma(yt[:, :, 0:Ng], S["YTL"].rearrange("(k p) t -> p k t", p=128)[:, :, T0 - NCTX:T0 - NCTX + Ng],
                       reads=[db("YTL")], writes=[b_yt])
            kb.dma(ga[:, :, 0:Ng], S["GAT"].rearrange("(k p) t -> p k t", p=128)[:, :, T0:T0 + Ng], reads=[db("GAT")], writes=[b_ga])
            kb.dma(gm[:, :, 0:Ng], S["GMT"].rearrange("(k p) t -> p k t", p=128)[:, :, T0:T0 + Ng], reads=[db("GMT")], writes=[b_gm])
            y = yt[:, :, 0:Ng]
            kb.tt("dve", gt[:, :, 0:Ng], y, y, ALU.mult, [b_yt], [b_gt])
            kb.ts("dve", gt[:, :, 0:Ng], gt[:, :, 0:Ng], 0.044715, 1.0, ALU.mult, ALU.add, [b_gt], [b_gt])
            kb.tt("dve", gt[:, :, 0:Ng], gt[:, :, 0:Ng], y, ALU.mult, [b_gt, b_yt], [b_gt])
            kb.act(sgm[:, :, 0:Ng], gt[:, :, 0:Ng], AF.Sigmoid, [b_gt], [b_sgm], scale=1.5957691216057308)
            kb.tt("dve", s1[:, :, 0:Ng], y, sgm[:, :, 0:Ng], ALU.mult, [b_yt, b_sgm], [b_s1])
            for of in range(4):
                pz, b_pz = pab.next()
                for k in range(4):
                    kb.mm(pz[:, 0:Ng], wglu[:, k, of * 128:(of + 1) * 128], s1[:, k, 0:Ng], k == 0, k == 3, [b_wglu, b_s1], [b_pz])
                sg, b_sg = sigr.next()
                kb.act(sg[:, 0:Ng], pz[:, 0:Ng], AF.Sigmoid, [b_pz, b_vpt], [b_sg], bias=vpt[:, 64 + of:65 + of], scale=1.0)
                kb.tt("dve", s2[:, of, 0:Ng], s1[:, of, 0:Ng], sg[:, 0:Ng], ALU.mult, [b_s1, b_sg], [b_s2])
            for of in range(8):
                fs = slice(of * 128, (of + 1) * 128)
                pa, b_pa = pab.next()
                for k in range(4):
                    kb.mm(pa[:, 0:Ng], wgp[:, k, fs], og[:, k, 0:Ng], k == 0, k == 3, [b_wgp, b_og], [b_pa])
                pb, b_pb = pab.next()
                for k in range(4):
                    rhs = s2[:, k, 0:Ng]
                    if isctx:
                        rhs = rhs.rearrange("p (b a c) -> p c a b", b=8, a=8, c=4)
                    kb.mm(pb[:, 0:Ng], ws5[:, k, fs], rhs, k == 0, k == 3, [b_ws5, b_s2], [b_pb])
                t1, b_t1 = t1r.next()
                t2, b_t2 = t2r.next()
                kb.tt("dve", t1[:, 0:Ng], pa[:, 0:Ng], ga[:, of, 0:Ng], ALU.mult, [b_pa, b_ga], [b_t1])
                kb.tt("dve", t2[:, 0:Ng], pb[:, 0:Ng], gm[:, of, 0:Ng], ALU.mult, [b_pb, b_gm], [b_t2])
                kb.tt("pool", mT[:, of, 0:Ng], t1[:, 0:Ng], t2[:, 0:Ng], ALU.add, [b_t1, b_t2], [b_mT])
            gi = 2 if isctx else 0
            Ai = 6 if isctx else 4
            for j in range(nt):
                i = i0 + j
                cs = slice(j * 128, (j + 1) * 128)
                pm, b_pm = pml.next()
                for half in range(2):
                    for k in range(KT):
                        kb.mm(pm[:, half * 512:(half + 1) * 512], mT[:, k, cs], wout[:, k, half * 512:(half + 1) * 512],
                              k == 0, k == KT - 1, [b_mT, b_wout], [b_pm])
                lt, b_lt = ltr.next()
                kb.dma(lt[:], S["LAT"][i * 128:(i + 1) * 128, :], reads=[db("LAT", i)], writes=[b_lt])
                tmp, b_tmp = tmpr.next()
                for half in range(2):
                    hs = slice(half * 512, (half + 1) * 512)
                    kb.tt("dve", tmp[:, hs], pm[:, hs], gbc[:, gi, hs], ALU.mult, [b_pm, b_gbc], [b_tmp])
                l2, b_l2 = l2r.next()
                kb.tt("pool", l2[:], tmp[:], lt[:], ALU.add, [b_tmp, b_lt], [b_l2])
                kb.dma(S["LAT"][i * 128:(i + 1) * 128, :], l2[:], reads=[b_l2], writes=[db("LAT", i)])
                xs2, b_xs2 = xs2r.next()
                ss, b_ss = ssr.next()
                kb.act(xs2[:], l2[:], AF.Square, [b_l2], [b_xs2, b_ss], scale=1.0 / 32.0, accum_out=ss[:, 0:1])
                kb.rstd(ss[:, 1:2], ss[:, 0:1], b_ss)
                kb.ts("dve", xs2[:], l2[:], ss[:, 1:2], None, ALU.mult, None, [b_l2, b_ss], [b_xs2])
                pt, b_pt = pt2.next()
                for k in range(KT):
                    kb.tr(pt[:, k, :], xs2[:, k * 128:(k + 1) * 128], identf[:], [b_xs2, b_identf], [b_pt])
                h2f, b_h2f = h2fr.next()
                for k in range(KT):
                    if k < 4:
                        kb.ts("dve", h2f[:, k, :], pt[:, k, :], ab[:, k, Ai:Ai + 1], ab[:, k, Ai + 1:Ai + 2], ALU.mult, ALU.add,
                              [b_pt, b_ab], [b_h2f])
                    else:
                        kb.act(h2f[:, k, :], pt[:, k, :], AF.Identity, [b_pt, b_ab], [b_h2f],
                               scale=ab[:, k, Ai:Ai + 1], bias=ab[:, k, Ai + 1:Ai + 2])
                h2b, b_h2b = h2br.next()
                kb.cp("pool", h2b[:], h2f[:], [b_h2f], [b_h2b])
                kb.dma(S["H2T"].rearrange("(k p) t -> p k t", p=128)[:, :, i * 128:(i + 1) * 128], h2b[:], reads=[b_h2b],
                       writes=[db("H2T")])
                if last:
                    pl_, b_pl = plg.next()
                    for k in range(KT):
                        kb.mm(pl_[:, 0:NEXP], h2f[:, k, :], wr[:, k, :], k == 0, k == KT - 1, [b_h2f, b_wr], [b_pl])
                    lg, b_lg = lgr.next()
                    ti = i - 2
                    kb.cp("dve", lg[:, 0, :], pl_[:, 0:NEXP], [b_pl], [b_lg])
                    kb.P.op("dve", lambda E, lg=lg: E.tensor_reduce(out=lg[:, 7, 0:1], in_=lg[:, 0, :], axis=mybir.AxisListType.X,
                                                                    op=ALU.max), [b_lg], [b_lg])
                    kb.ts("dve", lg[:, 1, :], lg[:, 0, :], lg[:, 7, 0:1], None, ALU.is_equal, None, [b_lg], [b_lg])
                    kb.stt("dve", lg[:, 2, :], lg[:, 1, :], -1.0e30, lg[:, 0, :], ALU.mult, ALU.add, [b_lg], [b_lg])
                    kb.P.op("dve", lambda E, lg=lg: E.tensor_reduce(out=lg[:, 7, 1:2], in_=lg[:, 2, :], axis=mybir.AxisListType.X,
                                                                    op=ALU.max), [b_lg], [b_lg])
                    kb.ts("dve", lg[:, 3, :], lg[:, 0, :], lg[:, 7, 1:2], None, ALU.is_ge, None, [b_lg], [b_lg])
                    kb.ts("dve", lg[:, 7, 2:3], lg[:, 7, 0:1], -1.0, None, ALU.mult, None, [b_lg], [b_lg])
                    kb.act(lg[:, 4, :], lg[:, 0, :], AF.Exp, [b_lg], [b_lg], bias=lg[:, 7, 2:3], scale=1.0)
                    kb.tt("dve", lg[:, 5, :], lg[:, 4, :], lg[:, 3, :], ALU.mult, [b_lg], [b_lg])
                    kb.P.op("dve", lambda E, lg=lg: E.tensor_reduce(out=lg[:, 7, 3:4], in_=lg[:, 5, :], axis=mybir.AxisListType.X,
                                                                    op=ALU.add), [b_lg], [b_lg])
                    kb.P.op("dve", lambda E, lg=lg: E.reciprocal(out=lg[:, 7, 4:5], in_=lg[:, 7, 3:4]), [b_lg], [b_lg])
                    kb.ts("dve", gates[:, ti, :], lg[:, 5, :], lg[:, 7, 4:5], None, ALU.mult, None, [b_lg], [b_gates])
        kb.P.barrier()


def phase_f(kb, I, S, OUT, l, C, last):
    db = kb.db
    gbc, b_gbc = C["gbc"]
    gates, b_gates = C["gates"]
    HBK = 256
    if last:
        sgroups = [list(range(2, 2 + NOWN // 128))]
        nexp, Hd = NEXP, H_MOE
    else:
        sgroups = [list(range(0, 17)), list(range(17, 34))]
        nexp, Hd = 1, H_FFN
    nblk = Hd // HBK
    with ExitStack() as es:
        h2, b_h2 = kb.sb(es, "h2", [128, KT, 17 * 128], BF16)
        facc, b_facc = kb.sb(es, "facc", [128, 17, D], F32)
        wgr = kb.sbr(es, "wg", [128, KT, HBK], BF16, 3)
        wur = kb.sbr(es, "wu", [128, KT, HBK], BF16, 3)
        wdr = kb.sbr(es, "wd", [128, HBK // 128, D], BF16, 3)
        sgr = kb.sbr(es, "sgf", [128, 512], F32, 2)
        ar = kb.sbr(es, "aact", [128, HBK // 128, 512], BF16, 3)
        ltr = kb.sbr(es, "ltf", [128, D], F32, 2)
        tmr = kb.sbr(es, "tmf", [128, D], F32, 2)
        otr = kb.sbr(es, "otf", [128, D], F32, 2)
        ssr = kb.sbr(es, "ssf", [128, 2], F32, 4)
        pgu = kb.psr(es, "pgu", [128, 512], 4)
        pdn = kb.psr(es, "pdn", [128, 512], 4)
        for tl in sgroups:
            n = len(tl)
            t0 = tl[0]
            kb.dma(h2[:, :, 0:n * 128], S["H2T"].rearrange("(k p) t -> p k t", p=128)[:, :, t0 * 128:(t0 + n) * 128],
                   reads=[db("H2T")], writes=[b_h2])
            tgs = [list(range(a, min(a + 4, n))) for a in range(0, n, 4)]
            items = []
            for e in range(nexp):
                for blk in range(nblk):
                    for tg in tgs:
                        items.append((e, blk, tg))
            wcur = {}

            def stage_a(item):
                e, blk, tg = item
                if (e, blk) not in wcur:
                    if last:
                        Wg, Wu, Wd = I["moe_w_gate"][0, e], I["moe_w_up"][0, e], I["moe_w_down"][0, e]
                    else:
                        Wg, Wu, Wd = I["ffn_w_gate"][0], I["ffn_w_up"][0], I["ffn_w_down"][0]
                    Wgv = Wg.rearrange("(k p) n -> p k n", p=128)
                    Wuv = Wu.rearrange("(k p) n -> p k n", p=128)
                    Wdv = Wd.rearrange("(c p) n -> p c n", p=128)
                    wg, b_wg = wgr.next()
                    wu, b_wu = wur.next()
                    wd, b_wd = wdr.next()
                    kb.dma(wg[:], Wgv[:, :, blk * HBK:(blk + 1) * HBK], writes=[b_wg], q="pool")
                    kb.dma(wu[:], Wuv[:, :, blk * HBK:(blk + 1) * HBK], writes=[b_wu], q="pool")
                    kb.dma(wd[:], Wdv[:, blk * (HBK // 128):(blk + 1) * (HBK // 128), :], writes=[b_wd], q="pool")
                    wcur.clear()
                    wcur[(e, blk)] = (wg, b_wg, wu, b_wu, wd, b_wd)
                wg, b_wg, wu, b_wu, wd, b_wd = wcur[(e, blk)]
                ntg = len(tg) * 128
                c0 = tg[0] * 128
                a, b_a = ar.next()
                for jc in range(HBK // 128):
                    js = slice(jc * 128, (jc + 1) * 128)
                    pg_, b_pg = pgu.next()
                    for k in range(KT):
                        kb.mm(pg_[:, 0:ntg], wg[:, k, js], h2[:, k, c0:c0 + ntg], k == 0, k == KT - 1, [b_wg, b_h2], [b_pg])
                    pu_, b_pu = pgu.next()
                    for k in range(KT):
                        kb.mm(pu_[:, 0:ntg], wu[:, k, js], h2[:, k, c0:c0 + ntg], k == 0, k == KT - 1, [b_wu, b_h2], [b_pu])
                    sg, b_sg = sgr.next()
                    kb.act(sg[:, 0:ntg], pg_[:, 0:ntg], AF.Silu, [b_pg], [b_sg])
                    kb.tt("dve", a[:, jc, 0:ntg], pu_[:, 0:ntg], sg[:, 0:ntg], ALU.mult, [b_pu, b_sg], [b_a])
                return (a, b_a, wd, b_wd)

            def stage_b(item, st, first):
                e, blk, tg = item
                a, b_a, wd, b_wd = st
                for ti_, t in enumerate(tg):
                    for half in range(2):
                        hs = slice(half * 512, (half + 1) * 512)
                        pd, b_pd = pdn.next()
                        for jc in range(HBK // 128):
                            kb.mm(pd[:], a[:, jc, ti_ * 128:(ti_ + 1) * 128], wd[:, jc, hs], jc == 0, jc == HBK // 128 - 1,
                                  [b_a, b_wd], [b_pd])
                        if last:
                            gw = gates[:, tl[t] - 2, e:e + 1]
                            if first:
                                kb.ts("dve", facc[:, t, hs], pd[:], gw, None, ALU.mult, None, [b_pd, b_gates], [b_facc])
                            else:
                                kb.stt("dve", facc[:, t, hs], pd[:], gw, facc[:, t, hs], ALU.mult, ALU.add,
                                       [b_pd, b_gates, b_facc], [b_facc])
                        else:
                            if first:
                                kb.cp("dve", facc[:, t, hs], pd[:], [b_pd], [b_facc])
                            else:
                                kb.tt("dve", facc[:, t, hs], pd[:], facc[:, t, hs], ALU.add, [b_pd, b_facc], [b_facc])

            stn = stage_a(items[0])
            for ii, item in enumerate(items):
                stc = stn
                if ii + 1 < len(items):
                    stn = stage_a(items[ii + 1])
                stage_b(item, stc, first=(item[0] == 0 and item[1] == 0))
            for t, i in enumerate(tl):
                lt, b_lt = ltr.next()
                kb.dma(lt[:], S["LAT"][i * 128:(i + 1) * 128, :], reads=[db("LAT", i)], writes=[b_lt])
                gi = 3 if i < 2 else 1
                tm, b_tm = tmr.next()
                kb.tt("dve", tm[:], facc[:, t, :], gbc[:, gi, :], ALU.mult, [b_facc, b_gbc], [b_tm])
                kb.tt("pool", tm[:], tm[:], lt[:], ALU.add, [b_tm, b_lt], [b_tm])
                if not last:
                    kb.dma(S["LAT"][i * 128:(i + 1) * 128, :], tm[:], reads=[b_tm], writes=[db("LAT", i)])
                else:
                    ot, b_ot = otr.next()
                    ss, b_ss = ssr.next()
                    kb.act(ot[:], tm[:], AF.Square, [b_tm], [b_ot, b_ss], scale=1.0 / 32.0, accum_out=ss[:, 0:1])
                    kb.rstd(ss[:, 1:2], ss[:, 0:1], b_ss)
                    kb.stt("dve", ot[:], tm[:], ss[:, 1:2], gbc[:, 4, :], ALU.mult, ALU.mult, [b_tm, b_ss, b_gbc], [b_ot])
                    kb.dma(OUT[(i - 2) * 128:(i - 1) * 128, :], ot[:], reads=[b_ot], writes=[db("OUT")])
        kb.P.barrier()


def _consts():
    s = np.arange(128)[:, None]
    t = np.arange(128)[None, :]
    same = (s // 64) == (t // 64)
    c = {}
    c["k_ident"] = np.eye(128, dtype=np.float32)
    c["k_uf"] = np.where(same & (s <= t), -1.0 / 16.0, 0.0).astype(np.float32)
    c["k_ub"] = np.where(same & (s >= t), -1.0 / 16.0, 0.0).astype(np.float32)
    c["k_amf"] = (same & (s <= t)).astype(np.float32)
    c["k_amb"] = (same & (s >= t)).astype(np.float32)
    a = np.zeros((2, 128), np.float32)
    a[0, :64] = 1.0
    a[1, 64:] = 1.0
    b = np.zeros((2, 128), np.float32)
    b[0, 64:] = -200.0
    b[1, :64] = -200.0
    c["k_mba"], c["k_mbb"] = a, b
    j = np.arange(64)
    s8 = np.arange(8)
    nf = np.concatenate([j + 1, -s8 - 1, 63 - j, [64]]).astype(np.float32)
    nb = np.concatenate([8 * (j // 8) + 8 - (j % 8), s8 - 8, j, [64]]).astype(np.float32)
    c["k_nvec"] = np.stack([nf, nb])
    slo = (np.arange(128) // 16)[:, None]
    tlo = (np.arange(128) // 16)[None, :]
    c["k_mge"] = (tlo >= slo).astype(np.float32)
    c["k_mle"] = (tlo <= slo).astype(np.float32)
    return c


_SWAP = [("gla_lr_f", "gla_lr_b"), ("gla_bias_f", "gla_bias_b"), ("s5_a_re_f", "s5_a_re_b"),
         ("s5_a_im_f", "s5_a_im_b"), ("s5_log_dt_f", "s5_log_dt_b")]
_WNAMES = ["w_mod", "b_mod", "norm1_w", "norm2_w", "final_norm_w", "w_in", "gla_lr_f", "gla_lr_b", "gla_bias_f",
           "gla_bias_b", "gla_norm_w", "s5_a_re_f", "s5_a_im_f", "s5_log_dt_f", "s5_a_re_b", "s5_a_im_b", "s5_log_dt_b",
           "s5_b_re", "s5_b_im", "s5_c_re", "s5_c_im", "s5_d", "s5_w_glu", "s5_b_glu", "w_gla_proj", "w_s5_proj",
           "w_out", "ffn_w_gate", "ffn_w_up", "ffn_w_down", "moe_router", "moe_w_gate", "moe_w_up", "moe_w_down"]


def prep_inputs(inputs):
    f = lambda a: np.ascontiguousarray(np.asarray(a, dtype=np.float32))
    W = {n: f(inputs[n]) for n in _WNAMES}
    Wodd = dict(W)
    for a, b in _SWAP:
        Wodd[a], Wodd[b] = W[b], W[a]
    wi = W["w_in"].copy()
    wi[:, :, OZF:OZF + 16] = W["w_in"][:, :, OZB:OZB + 16]
    wi[:, :, OZB:OZB + 16] = W["w_in"][:, :, OZF:OZF + 16]
    Wodd["w_in"] = wi
    cst = _consts()
    x, ctx, c, c_ctx = f(inputs["x"]), f(inputs["ctx"]), f(inputs["c"]), f(inputs["c_ctx"])
    maps = []
    for core in range(8):
        b, p = core // 2, core % 2
        m = dict(Wodd if p else W)
        m.update(cst)
        xb, cb = x[b], ctx[b]
        if p:
            xb, cb = xb[::-1], cb[::-1]
        m["x"] = np.ascontiguousarray(xb)
        m["ctx"] = np.ascontiguousarray(cb)
        cc = np.stack([c[b], c_ctx], axis=-1).reshape(KT, 128, 2).transpose(1, 0, 2)
        m["cc"] = np.ascontiguousarray(cc)
        maps.append(m)
    return maps


_NC_CACHE = {}


def kernel(**inputs):
    if "kb" not in _NC_CACHE:
        _NC_CACHE["kb"] = build()
    kb = _NC_CACHE["kb"]
    maps = prep_inputs(inputs)
    maps = [{k: v for k, v in m.items() if k in kb.ins} for m in maps]
    res = run_bass_kernel_spmd(kb.nc, maps, core_ids=list(range(8)))
    out = np.empty((4, NLAT, D), np.float32)
    for core in range(8):
        b, p = core // 2, core % 2
        o = np.asarray(res.results[core]["out"], dtype=np.float32)
        if p:
            out[b, NLAT - NOWN:] = o[::-1]
        else:
            out[b, :NOWN] = o
    return out
```

```python
import math
from contextlib import ExitStack

import numpy as np
import concourse.bass as bass
import concourse.mybir as mybir
from concourse.bass_utils import run_bass_kernel_spmd

F32 = mybir.dt.float32
BF16 = mybir.dt.bfloat16
AF = mybir.ActivationFunctionType
ALU = mybir.AluOpType

D = 1024
KT = 8
NCTX = 256
NLAT = 4096
NTOK = NCTX + NLAT
NTILE = NTOK // 128
H_FFN = 2816
H_MOE = 3584
NEXP = 8
NOWN = 2048
EPS = 1e-6
WIN = 4128
OK_, OV_, OZF, OZB, OU_, OQ_, OR_, OGA, OGM = 0, 256, 768, 784, 800, 1312, 1568, 2080, 3104
NVEC = 137


class Buf:
    __slots__ = ("name", "lws", "rd", "gid")

    def __init__(self, name):
        self.name = name
        self.lws = []
        self.rd = {}
        self.gid = None


class Prog:
    ENGS = ("pe", "act", "dve", "pool", "sp")
    NDMASEM = 12

    def __init__(self, nc, es):
        self.nc = nc
        self.streams = {e: [] for e in self.ENGS}
        self.cnt = {}
        self.known = {e: {} for e in self.ENGS}
        self.sems = {}
        for e in ("pe", "act", "dve", "pool"):
            self.sems[e] = es.enter_context(nc.semaphore("s_" + e))
            self.cnt[e] = 0
        self.dma_rr = {"sp": 0, "pool": 0}
        for q in ("sp", "pool"):
            for i in range(self.NDMASEM):
                s = f"d_{q}{i}"
                self.sems[s] = es.enter_context(nc.semaphore(s))
                self.cnt[s] = 0
        self.last_dma = []
        self.ninst = 0

    def _need(self, eng, stream, seq):
        k = self.known[eng]
        if k.get(stream, -1) >= seq:
            return
        k[stream] = seq
        sem = self.sems[stream]
        val = (seq + 1) * (16 if stream.startswith("d_") else 1)
        self.streams[eng].append(lambda E, sem=sem, val=val: E.wait_ge(sem, val))
        self.ninst += 1

    def _deps(self, eng, reads, writes, gid=None):
        for b in reads:
            for lw in b.lws:
                if not (eng == "pe" and lw[0] == "pe"):
                    self._need(eng, *lw)
        for b in writes:
            if gid is not None and b.gid == gid:
                continue
            for lw in b.lws:
                if lw[0] != eng:
                    self._need(eng, *lw)
            for st, sq in b.rd.items():
                if st != eng:
                    self._need(eng, st, sq)

    def _mark(self, stream, seq, reads, writes, gid=None):
        for b in reads:
            if b.rd.get(stream, -1) < seq:
                b.rd[stream] = seq
        for b in writes:
            if gid is not None and b.gid == gid:
                b.lws.append((stream, seq))
            else:
                b.lws = [(stream, seq)]
                b.rd = {}
                b.gid = gid

    def op(self, eng, fn, reads=(), writes=()):
        self._deps(eng, reads, writes)
        seq = self.cnt[eng]
        self.cnt[eng] = seq + 1
        sem = self.sems[eng]
        self.streams[eng].append(lambda E, fn=fn, sem=sem: fn(E).then_inc(sem, 1))
        self._mark(eng, seq, reads, writes)
        self.ninst += 1

    def dma(self, q, out, in_, reads=(), writes=(), gid=None, **kw):
        slot = self.dma_rr[q]
        self.dma_rr[q] = (slot + 1) % self.NDMASEM
        stream = f"d_{q}{slot}"
        seq = self.cnt[stream]
        if seq > 0:
            self._need(q, stream, seq - 1)
        self._deps(q, reads, writes, gid)
        self.cnt[stream] = seq + 1
        sem = self.sems[stream]
        self.streams[q].append(
            lambda E, out=out, in_=in_, sem=sem, kw=kw: E.dma_start(out=out, in_=in_, **kw).then_inc(sem, 16))
        self._mark(stream, seq, reads, writes, gid)
        self.ninst += 1

    def barrier(self):
        self.nbar = getattr(self, "nbar", 0) + 1
        for e in self.ENGS:
            for st, c in self.cnt.items():
                if c > 0 and not (st == e and e == "sp"):
                    self._need(e, st, c - 1)

    def wait_all_dma(self, eng="sp"):
        for s, c in self.cnt.items():
            if s.startswith("d_") and c > 0:
                self._need(eng, s, c - 1)

    def emit(self):
        nc = self.nc
        with nc.Block() as block:
            @block.sync
            def _(E):
                for t in self.streams["sp"]:
                    t(E)

            @block.tensor
            def _(E):
                for t in self.streams["pe"]:
                    t(E)

            @block.scalar
            def _(E):
                for t in self.streams["act"]:
                    t(E)

            @block.vector
            def _(E):
                for t in self.streams["dve"]:
                    t(E)

            @block.gpsimd
            def _(E):
                for t in self.streams["pool"]:
                    t(E)


class Ring:
    def __init__(self, items):
        self.items = items
        self.i = 0

    def next(self):
        it = self.items[self.i % len(self.items)]
        self.i += 1
        return it


class KB:
    def __init__(self, dbg=(), stop_after=None, nlayers=2):
        self.nc = bass.Bass("TRN2", target_bir_lowering=False)
        self.es = ExitStack()
        self.P = Prog(self.nc, self.es)
        self.dbg = set(dbg)
        self.stop_after = stop_after
        self.nlayers = nlayers
        self.dbufs = {}
        self.ins = {}
        self.uid = 0

    def inp(self, name, shape, dt=F32):
        ap = self.nc.dram_tensor(name, list(shape), dt, kind="ExternalInput").ap()
        self.ins[name] = ap
        return ap

    def scratch(self, name, shape, dt, out=False):
        kind = "ExternalOutput" if (out or name in self.dbg) else "Internal"
        return self.nc.dram_tensor(name, list(shape), dt, kind=kind).ap()

    def db(self, name, idx=0):
        k = (name, idx)
        if k not in self.dbufs:
            self.dbufs[k] = Buf(f"DRAM:{name}:{idx}")
        return self.dbufs[k]

    def sb(self, es, name, shape, dt):
        self.uid += 1
        t = es.enter_context(self.nc.sbuf_tensor(f"{name}_{self.uid}", list(shape), dt))
        return t, Buf(name)

    def sbr(self, es, name, shape, dt, n):
        return Ring([self.sb(es, f"{name}{i}", shape, dt) for i in range(n)])

    def ps(self, es, name, shape, dt=F32):
        self.uid += 1
        t = es.enter_context(self.nc.psum_tensor(f"{name}_{self.uid}", list(shape), dt))
        return t, Buf(name)

    def psr(self, es, name, shape, n, dt=F32):
        return Ring([self.ps(es, f"{name}{i}", shape, dt) for i in range(n)])

    def mm(self, out, lhsT, rhs, start, stop, reads, writes):
        self.P.op("pe", lambda E: E.matmul(out, lhsT=lhsT, rhs=rhs, start=start, stop=stop), reads, writes)

    def tr(self, out, in_, ident, reads, writes):
        self.P.op("pe", lambda E: E.transpose(out, in_, ident), reads, writes)

    def act(self, out, in_, func, reads, writes, **kw):
        self.P.op("act", lambda E: E.activation(out=out, in_=in_, func=func, **kw), reads, writes)

    def tt(self, eng, out, in0, in1, op, reads, writes):
        self.P.op(eng, lambda E: E.tensor_tensor(out=out, in0=in0, in1=in1, op=op), reads, writes)

    def ts(self, eng, out, in0, s1, s2, op0, op1, reads, writes):
        if s2 is None:
            self.P.op(eng, lambda E: E.tensor_scalar(out=out, in0=in0, scalar1=s1, scalar2=None, op0=op0), reads, writes)
        else:
            self.P.op(eng, lambda E: E.tensor_scalar(out=out, in0=in0, scalar1=s1, scalar2=s2, op0=op0, op1=op1),
                      reads, writes)

    def dump(self, name, ap, reads):
        if ("dump_" + name) not in self.dbg:
            return
        if not hasattr(self, "_dumped"):
            self._dumped = set()
        if name in self._dumped:
            return
        self._dumped.add(name)
        d = self.nc.dram_tensor("dump_" + name, list(ap.shape), ap.dtype, kind="ExternalOutput").ap()
        self.dma(d, ap, reads=reads, writes=[self.db("dump_" + name)])

    def rstd(self, out, in_, b):
        self.act(out, in_, AF.Sqrt, [b], [b], bias=EPS, scale=1.0)
        self.P.op("dve", lambda E: E.reciprocal(out=out, in_=out), [b], [b])

    def stt(self, eng, out, in0, scalar, in1, op0, op1, reads, writes):
        self.P.op(eng, lambda E: E.scalar_tensor_tensor(out=out, in0=in0, scalar=scalar, in1=in1, op0=op0, op1=op1),
                  reads, writes)

    def cp(self, eng, out, in_, reads, writes):
        if eng == "act":
            self.P.op("act", lambda E: E.copy(out=out, in_=in_), reads, writes)
        else:
            self.P.op(eng, lambda E: E.tensor_copy(out=out, in_=in_), reads, writes)

    def memset(self, eng, ap, val, writes):
        self.P.op(eng, lambda E: E.memset(ap, val), (), writes)

    def dma(self, out, in_, reads=(), writes=(), q="sp", gid=None, **kw):
        if gid is None and len(writes) == 1 and writes[0].name.startswith("DRAM:") and not writes[0].name.startswith("DRAM:LAT"):
            gid = (writes[0].name, getattr(self.P, "nbar", 0))
        self.P.dma(q, out, in_, reads, writes, gid=gid, **kw)


def build(dbg=(), stop_after=None, nlayers=2, hook=None):
    kb = KB(dbg, stop_after, nlayers)
    nc, P = kb.nc, kb.P
    shapes = {"x": [NLAT, D], "ctx": [NCTX, D], "cc": [128, KT, 2]}
    for n, s_ in [("w_mod", [2, D, 6 * D]), ("b_mod", [2, 6 * D]), ("norm1_w", [2, D]), ("norm2_w", [2, D]),
                 ("final_norm_w", [D]), ("w_in", [2, D, WIN]), ("gla_lr_f", [2, 16, 256]), ("gla_lr_b", [2, 16, 256]),
                 ("gla_bias_f", [2, 256]), ("gla_bias_b", [2, 256]), ("gla_norm_w", [2, 128]),
                 ("s5_a_re_f", [2, 32, 64]), ("s5_a_im_f", [2, 32, 64]), ("s5_log_dt_f", [2, 32]),
                 ("s5_a_re_b", [2, 32, 64]), ("s5_a_im_b", [2, 32, 64]), ("s5_log_dt_b", [2, 32]),
                 ("s5_b_re", [2, 32, 64, 16]), ("s5_b_im", [2, 32, 64, 16]), ("s5_c_re", [2, 32, 16, 64]),
                 ("s5_c_im", [2, 32, 16, 64]), ("s5_d", [2, 512]), ("s5_w_glu", [2, 512, 512]), ("s5_b_glu", [2, 512]),
                 ("w_gla_proj", [2, 512, D]), ("w_s5_proj", [2, 512, D]), ("w_out", [2, D, D]),
                 ("ffn_w_gate", [1, D, H_FFN]), ("ffn_w_up", [1, D, H_FFN]), ("ffn_w_down", [1, H_FFN, D]),
                 ("moe_router", [1, D, NEXP]), ("moe_w_gate", [1, NEXP, D, H_MOE]), ("moe_w_up", [1, NEXP, D, H_MOE]),
                 ("moe_w_down", [1, NEXP, H_MOE, D]),
                 ("k_ident", [128, 128]), ("k_uf", [128, 128]), ("k_ub", [128, 128]), ("k_amf", [128, 128]),
                 ("k_amb", [128, 128]), ("k_mba", [2, 128]), ("k_mbb", [2, 128]), ("k_nvec", [2, NVEC]),
                 ("k_mge", [128, 128]), ("k_mle", [128, 128])]:
        shapes[n] = s_

    class LazyIn(dict):
        def __missing__(self, n):
            ap = kb.inp(n, shapes[n])
            self[n] = ap
            return ap

    I = LazyIn()
    OUT = nc.dram_tensor("out", [NOWN, D], F32, kind="ExternalOutput").ap()

    S = {}
    S["LAT"] = kb.scratch("LAT", [NTOK, D], F32)
    S["V"] = kb.scratch("V", [NTOK, 512], BF16)
    S["GR"] = kb.scratch("GR", [NTOK, 512], BF16)
    S["GAT"] = kb.scratch("GAT", [D, NTOK], BF16)
    S["GMT"] = kb.scratch("GMT", [D, NTOK], BF16)
    S["UTL"] = kb.scratch("UTL", [512, NLAT], BF16)
    S["UTC"] = kb.scratch("UTC", [512, NCTX], BF16)
    S["KD"] = kb.scratch("KD", [NTILE, 2, 128, 512], BF16)
    S["KQT"] = kb.scratch("KQT", [NTILE, 128, 2, 4, 2, 128], BF16)
    S["OGT"] = kb.scratch("OGT", [512, NTOK], BF16)
    S["YTL"] = kb.scratch("YTL", [512, NLAT], F32)
    S["YTC"] = kb.scratch("YTC", [512, NCTX], F32)
    S["H2T"] = kb.scratch("H2T", [D, NTOK], BF16)
    db = kb.db

    es0 = kb.es
    identf, b_identf = kb.sb(es0, "identf", [128, 128], F32)
    identb, b_identb = kb.sb(es0, "identb", [128, 128], BF16)
    ones, b_ones = kb.sb(es0, "ones", [128, 128], F32)
    uf, b_uf = kb.sb(es0, "uf", [128, 128], F32)
    ub, b_ub = kb.sb(es0, "ub", [128, 128], F32)
    amf, b_amf = kb.sb(es0, "amf", [128, 128], F32)
    amb, b_amb = kb.sb(es0, "amb", [128, 128], F32)
    mba, b_mba = kb.sb(es0, "mba", [2, 128], F32)
    mbb, b_mbb = kb.sb(es0, "mbb", [2, 128], F32)
    dec, b_dec = kb.sb(es0, "dec", [128, NTILE, 2, 2, 4], F32)
    modT, b_modT = kb.sb(es0, "modT", [128, 48, 2], F32)
    vpt, b_vpt = kb.sb(es0, "vpt", [128, 76], F32)
    ab, b_ab = kb.sb(es0, "ab", [128, KT, 8], F32)
    gbc, b_gbc = kb.sb(es0, "gbc", [128, 5, D], F32)
    gnw, b_gnw = kb.sb(es0, "gnw", [128, 1], F32)
    scT, b_scT = kb.sb(es0, "scT", [128, KT, 2], F32)
    gates, b_gates = kb.sb(es0, "gates", [128, NOWN // 128, NEXP], F32)

    for t, b, src in [(identf, b_identf, "k_ident"), (uf, b_uf, "k_uf"), (ub, b_ub, "k_ub"), (amf, b_amf, "k_amf"),
                      (amb, b_amb, "k_amb"), (mba, b_mba, "k_mba"), (mbb, b_mbb, "k_mbb")]:
        kb.dma(t[:], I[src], writes=[b])
    kb.cp("dve", identb[:], identf[:], [b_identf], [b_identb])
    kb.memset("dve", ones[:], 1.0, [b_ones])
    kb.dma(scT[:], I["cc"], writes=[b_scT])
    kb.act(scT[:], scT[:], AF.Silu, [b_scT], [b_scT])
    for i in range(NTILE):
        src = I["ctx"][i * 128:(i + 1) * 128, :] if i < 2 else I["x"][(i - 2) * 128:(i - 1) * 128, :]
        kb.dma(S["LAT"][i * 128:(i + 1) * 128, :], src, writes=[db("LAT", i)])

    CC = dict(identf=(identf, b_identf), identb=(identb, b_identb), ones=(ones, b_ones),
              uf=(uf, b_uf), ub=(ub, b_ub), amf=(amf, b_amf), amb=(amb, b_amb),
                                     mba=(mba, b_mba), mbb=(mbb, b_mbb), dec=(dec, b_dec), modT=(modT, b_modT),
                                     vpt=(vpt, b_vpt), ab=(ab, b_ab), gbc=(gbc, b_gbc), gnw=(gnw, b_gnw),
                                     scT=(scT, b_scT), gates=(gates, b_gates))
    for l in range(nlayers):
        layer(kb, I, S, OUT, l, CC)
        if kb.stop_after is not None and kb.stop_after[0] == l:
            break
    if hook is not None:
        hook(kb, CC, S)
    P.wait_all_dma("sp")
    P.emit()
    kb.es.close()
    return kb


def layer(kb, I, S, OUT, l, C):
    last = (l == kb.nlayers - 1) and kb.nlayers == 2
    stop = kb.stop_after[1] if (kb.stop_after is not None and kb.stop_after[0] == l) else None
    phase_mod(kb, I, l, C)
    if stop == "mod":
        return
    phase_a(kb, I, S, l, C)
    if stop == "a":
        return
    phase_g(kb, I, S, l, C)
    if stop == "g":
        return
    phase_s5(kb, I, S, l, C, last)
    if stop == "s5":
        return
    phase_m(kb, I, S, l, C, last)
    if stop == "m":
        return
    phase_f(kb, I, S, OUT, l, C, last)


def phase_mod(kb, I, l, C):
    db = kb.db
    identf, b_identf = C["identf"]
    ones, b_ones = C["ones"]
    modT, b_modT = C["modT"]
    vpt, b_vpt = C["vpt"]
    ab, b_ab = C["ab"]
    gbc, b_gbc = C["gbc"]
    gnw, b_gnw = C["gnw"]
    scT, b_scT = C["scT"]
    with ExitStack() as es:
        vrow, b_vrow = kb.sb(es, "vrow", [76, 128], F32)
        wmr = kb.sbr(es, "wm", [128, KT, 512], F32, 2)
        dg, b_dg = kb.sb(es, "dg", [128, 4, 128], F32)
        pmod, b_pmod = kb.ps(es, "pmod", [128, 96])
        ptr, b_ptr = kb.ps(es, "ptrm", [128, 128])
        pbc = kb.psr(es, "pbc", [128, 512], 2)
        kb.dma(vrow[0:48, :], I["b_mod"][l].rearrange("(j p) -> j p", p=128), writes=[b_vrow])
        kb.dma(vrow[48:56, :], I["norm1_w"][l].rearrange("(j p) -> j p", p=128), writes=[b_vrow])
        kb.dma(vrow[56:64, :], I["norm2_w"][l].rearrange("(j p) -> j p", p=128), writes=[b_vrow])
        kb.dma(vrow[64:68, :], I["s5_b_glu"][l].rearrange("(j p) -> j p", p=128), writes=[b_vrow])
        kb.dma(vrow[68:76, :], I["final_norm_w"].rearrange("(j p) -> j p", p=128), writes=[b_vrow])
        kb.dma(gnw[:], I["gla_norm_w"][l].rearrange("(p o) -> p o", o=1), writes=[b_gnw])
        kb.tr(ptr[:, 0:76], vrow[0:76, :], identf[0:76, 0:76], [b_vrow, b_identf], [b_ptr])
        kb.cp("dve", vpt[:], ptr[:, 0:76], [b_ptr], [b_vpt])
        wm_src = I["w_mod"][l].rearrange("(k p) n -> p k n", p=128)
        for blk in range(12):
            wm, b_wm = wmr.next()
            kb.dma(wm[:], wm_src[:, :, blk * 512:(blk + 1) * 512], writes=[b_wm])
            for jj in range(4):
                j = blk * 4 + jj
                for k in range(KT):
                    kb.mm(pmod[:, 2 * j:2 * j + 2], wm[:, k, jj * 128:(jj + 1) * 128], scT[:, k, :], k == 0, k == KT - 1,
                          [b_wm, b_scT], [b_pmod])
        kb.tt("dve", modT[:], pmod[:].rearrange("p (j w) -> p j w", w=2),
              vpt[:, 0:48].unsqueeze(2).to_broadcast([128, 48, 2]), ALU.add, [b_pmod, b_vpt], [b_modT])
        for w_, (sc_c, sh_c, nw_c) in enumerate([(8, 0, 48), (8, 0, 48), (32, 24, 56), (32, 24, 56)]):
            who = w_ % 2
            kb.stt("dve", ab[:, :, 2 * w_], modT[:, sc_c:sc_c + 8, who], 1.0, vpt[:, nw_c:nw_c + 8], ALU.add, ALU.mult,
                   [b_modT, b_vpt], [b_ab])
            kb.cp("dve", ab[:, :, 2 * w_ + 1], modT[:, sh_c:sh_c + 8, who], [b_modT], [b_ab])
        for gi, (c0, who) in enumerate([(16, 0), (40, 0), (16, 1), (40, 1), (68, -1)]):
            for half in range(2):
                pb, b_pb = pbc.next()
                for jj in range(4):
                    j = half * 4 + jj
                    col = modT[:, c0 + j, who:who + 1] if who >= 0 else vpt[:, c0 + j:c0 + j + 1]
                    kb.ts("dve", dg[:, jj, :], identf[:], col, None, ALU.mult, None,
                          [b_identf, b_modT, b_vpt], [b_dg])
                    kb.mm(pb[:, jj * 128:(jj + 1) * 128], ones[:], dg[:, jj, :], True, True, [b_ones, b_dg], [b_pb])
                kb.cp("act", gbc[:, gi, half * 512:(half + 1) * 512], pb[:], [b_pb], [b_gbc])
        kb.P.barrier()


def phase_a(kb, I, S, l, C):
    db = kb.db
    identf, b_identf = C["identf"]
    uf, b_uf = C["uf"]
    ub, b_ub = C["ub"]
    mba, b_mba = C["mba"]
    mbb, b_mbb = C["mbb"]
    dec, b_dec = C["dec"]
    ab, b_ab = C["ab"]
    with ExitStack() as es:
        win, b_win = kb.sb(es, "win", [128, KT, WIN], BF16)
        wkd, b_wkd = kb.sb(es, "wkd", [128, KT, 512], BF16)
        wqd, b_wqd = kb.sb(es, "wqd", [128, KT, 512], BF16)
        lrd = [kb.sb(es, "lrd%d" % d, [17, 512], F32) for d in range(2)]
        z1 = [kb.sb(es, "z1%d" % d, [17, 512], F32) for d in range(2)]
        xtr = kb.sbr(es, "xt", [128, D], F32, 2)
        ssr = kb.sbr(es, "ss", [128, 2], F32, 4)
        xs, b_xs = kb.sb(es, "xs", [128, 4, D], F32)
        hTr = kb.sbr(es, "hT", [128, KT, 512], BF16, 2)
        kq, b_kq = kb.sb(es, "kq", [128, 8, 512], F32)
        ust, b_ust = kb.sb(es, "ust", [128, 4, 512], BF16)
        gstr = kb.sbr(es, "gst", [128, 8, 512], BF16, 1)
        vstr = kb.sbr(es, "vst", [128, 512], BF16, 2)
        rstr = kb.sbr(es, "rst", [128, 512], BF16, 2)
        sp = [kb.sb(es, "sp%d" % d, [128, 512], F32) for d in range(2)]
        etmp, b_etmp = kb.sb(es, "etmp", [128, 512], F32)
        ektok, b_ektok = kb.sb(es, "ektok", [128, 512], F32)
        kdstr = kb.sbr(es, "kdst", [128, 2, 512], BF16, 1)
        kqstr = kb.sbr(es, "kqst", [128, 2, 4, 2, 128], BF16, 1)
        ekr = kb.sbr(es, "ek", [128, 128], F32, 2)
        eqr = kb.sbr(es, "eq", [128, 128], F32, 2)
        ptr = kb.psr(es, "ptr", [128, 512], 2)
        pfm = kb.psr(es, "pfm", [128, 512], 2)
        ptm = kb.psr(es, "ptm", [128, 512], 2)
        pg = kb.psr(es, "pg", [128, 512], 2)

        wsrc = I["w_in"][l].rearrange("(k p) n -> p k n", p=128)
        for k in range(KT):
            kb.dma(win[:, k, :], wsrc[:, k, :], writes=[b_win], q="pool", gid=("win", l))
        for r in range(2):
            o5 = wkd[:].rearrange("p k (h r d) -> p k h r d", h=4, r=2)[:, :, :, r, :]
            kb.cp("pool", o5, win[:, :, OK_:OK_ + 256].rearrange("p k (h d) -> p k h d", h=4), [b_win], [b_wkd])
            o5 = wqd[:].rearrange("p k (h r d) -> p k h r d", h=4, r=2)[:, :, :, r, :]
            kb.ts("pool", o5, win[:, :, OQ_:OQ_ + 256].rearrange("p k (h d) -> p k h d", h=4), 0.125, None, ALU.mult, None,
                  [b_win], [b_wqd])
        for d, (lrn, bn) in enumerate([("gla_lr_f", "gla_bias_f"), ("gla_lr_b", "gla_bias_b")]):
            t, b = lrd[d]
            for r in range(2):
                kb.dma(t[0:16, :].rearrange("k (h r d) -> k h r d", h=4, r=2)[:, :, r, :],
                       I[lrn][l].rearrange("k (h d) -> k h d", h=4), writes=[b])
                kb.dma(t[16:17, :].rearrange("k (h r d) -> k h r d", h=4, r=2)[:, :, r, :],
                       I[bn][l].rearrange("(o h d) -> o h d", o=1, h=4), writes=[b])
            kb.memset("dve", z1[d][0][:], 1.0, [z1[d][1]])

        groups = [(0, 2)] + [(2 + 4 * g, 4) for g in range(8)]
        for (i0, nt) in groups:
            Ng = 128 * nt
            isctx = i0 == 0
            Ai = 2 if isctx else 0
            need_out = (l == 0) or (2 <= i0 < 2 + NOWN // 128)
            need_q = need_out
            hT, b_hT = hTr.next()
            for j in range(nt):
                xt, b_xt = xtr.next()
                ss, b_ss = ssr.next()
                kb.dma(xt[:], S["LAT"][(i0 + j) * 128:(i0 + j + 1) * 128, :], reads=[db("LAT", i0 + j)], writes=[b_xt])
                kb.act(xs[:, j, :], xt[:], AF.Square, [b_xt], [b_xs, b_ss], scale=1.0 / 32.0, accum_out=ss[:, 0:1])
                kb.rstd(ss[:, 1:2], ss[:, 0:1], b_ss)
                kb.ts("dve", xs[:, j, :], xt[:], ss[:, 1:2], None, ALU.mult, None, [b_xt, b_ss], [b_xs])
            for k in range(KT):
                pt, b_pt = ptr.next()
                for j in range(nt):
                    kb.tr(pt[:, j * 128:(j + 1) * 128], xs[:, j, k * 128:(k + 1) * 128], identf[:], [b_xs, b_identf], [b_pt])
                if k % 2 == 0:
                    kb.ts("dve", hT[:, k, 0:Ng], pt[:, 0:Ng], ab[:, k, Ai:Ai + 1], ab[:, k, Ai + 1:Ai + 2], ALU.mult, ALU.add,
                          [b_pt, b_ab], [b_hT])
                else:
                    kb.act(hT[:, k, 0:Ng], pt[:, 0:Ng], AF.Identity, [b_pt, b_ab], [b_hT],
                           scale=ab[:, k, Ai:Ai + 1], bias=ab[:, k, Ai + 1:Ai + 2])

            kb.dump("xs", xs[:], [b_xs])
            kb.dump("hT", hT[:], [b_hT])
            kb.dump("win", win[:, 0, :], [b_win])
            def fm(W, bW, col0, M, perm=False):
                pf, b_pf = pfm.next()
                for k in range(KT):
                    rhs = hT[:, k, 0:Ng]
                    if perm:
                        rhs = rhs.rearrange("p (c a b) -> p b a c", c=4, a=8, b=8)
                    kb.mm(pf[0:M, 0:Ng], W[:, k, col0:col0 + M], rhs, k == 0, k == KT - 1, [bW, b_hT], [b_pf])
                return pf, b_pf

            ev = [0]

            def evac(out, in_, reads, writes):
                ev[0] += 1
                kb.cp("dve" if ev[0] % 2 else "act", out, in_, reads, writes)

            for ft in range(4):
                pf, b_pf = fm(win, b_win, OU_ + ft * 128, 128, perm=isctx)
                evac(ust[:, ft, 0:Ng], pf[:, 0:Ng], [b_pf], [b_ust])
            if isctx:
                kb.dma(S["UTC"].rearrange("(f p) t -> p f t", p=128), ust[:, :, 0:Ng], reads=[b_ust], writes=[db("UTC")])
            else:
                t0 = (i0 - 2) * 128
                kb.dma(S["UTL"].rearrange("(f p) t -> p f t", p=128)[:, :, t0:t0 + Ng], ust[:, :, 0:Ng], reads=[b_ust],
                       writes=[db("UTL")])
            if need_out:
                for nm, c0 in (("GAT", OGA), ("GMT", OGM)):
                    gst, b_gst = gstr.next()
                    for ft in range(8):
                        pf, b_pf = fm(win, b_win, c0 + ft * 128, 128)
                        kb.act(gst[:, ft, 0:Ng], pf[:, 0:Ng], AF.Sigmoid, [b_pf], [b_gst])
                    kb.dma(S[nm].rearrange("(f p) t -> p f t", p=128)[:, :, i0 * 128:i0 * 128 + Ng], gst[:, :, 0:Ng],
                           reads=[b_gst], writes=[db(nm)])
            for h in range(4):
                pf, b_pf = fm(wkd, b_wkd, h * 128, 128)
                evac(kq[:, h, 0:Ng], pf[:, 0:Ng], [b_pf], [b_kq])
            for h in range(4 if need_q else 0):
                pf, b_pf = fm(wqd, b_wqd, h * 128, 128)
                evac(kq[:, 4 + h, 0:Ng], pf[:, 0:Ng], [b_pf], [b_kq])
            for d, c0 in enumerate((OZF, OZB)):
                pf, b_pf = fm(win, b_win, c0, 16)
                evac(z1[d][0][0:16, 0:Ng], pf[0:16, 0:Ng], [b_pf], [z1[d][1]])

            for j in range(nt):
                i = i0 + j
                cs = slice(j * 128, (j + 1) * 128)

                def tm(W, bW, col0):
                    p_, b_ = ptm.next()
                    for k in range(KT):
                        kb.mm(p_[:, 0:512], hT[:, k, cs], W[:, k, col0:col0 + 512], k == 0, k == KT - 1, [bW, b_hT], [b_])
                    return p_, b_

                pv, b_pv = tm(win, b_win, OV_)
                vst, b_vst = vstr.next()
                evac(vst[:], pv[:], [b_pv], [b_vst])
                kb.dma(S["V"][i * 128:(i + 1) * 128, :], vst[:], reads=[b_vst], writes=[db("V")])
                if need_out:
                    pr, b_pr = tm(win, b_win, OR_)
                    rst, b_rst = rstr.next()
                    kb.act(rst[:], pr[:], AF.Silu, [b_pr], [b_rst])
                    kb.dma(S["GR"][i * 128:(i + 1) * 128, :], rst[:], reads=[b_rst], writes=[db("GR")])
                for d in range(2):
                    px, b_px = pg.next()
                    kb.mm(px[:, 0:512], z1[d][0][0:17, cs], lrd[d][0][0:17, :], True, True, [z1[d][1], lrd[d][1]], [b_px])
                    kb.act(etmp[:], px[:], AF.Exp, [b_px], [b_etmp], scale=-1.0)
                    kb.act(sp[d][0][:], etmp[:], AF.Ln, [b_etmp], [sp[d][1]], bias=1.0)
                pk, b_pk = tm(wkd, b_wkd, 0)
                kdst, b_kdst = kdstr.next()
                for d in range(2):
                    U, bU = (uf, b_uf) if d == 0 else (ub, b_ub)
                    pb_, b_pb = pg.next()
                    kb.mm(pb_[:, 0:512], U[:], sp[d][0][:], True, True, [bU, sp[d][1]], [b_pb])
                    kb.act(ektok[:], pb_[:], AF.Exp, [b_pb], [b_ektok], scale=-1.0)
                    kb.tt("dve", kdst[:, d, :], pk[:], ektok[:], ALU.mult, [b_pk, b_ektok], [b_kdst])
                kb.dma(S["KD"][i].rearrange("d p c -> p d c"), kdst[:], reads=[b_kdst], writes=[db("KD")])
                kqst, b_kqst = kqstr.next()
                for d in range(2):
                    U, bU = (uf, b_uf) if d == 0 else (ub, b_ub)
                    for h in range(4):
                        ph, b_ph = pg.next()
                        spd = sp[d][0][:, h * 128:(h + 1) * 128]
                        kb.mm(ph[:, 0:128], spd, U[:], True, True, [sp[d][1], bU], [b_ph])
                        if need_q:
                            kb.mm(ph[:, 128:256], spd, U[:], True, False, [sp[d][1], bU], [b_ph])
                            kb.mm(ph[:, 128:256], mba[0:2, :], mbb[0:2, :], False, True, [b_mba, b_mbb], [b_ph])
                        ek, b_ek = ekr.next()
                        eq, b_eq = eqr.next()
                        kb.act(ek[:], ph[:, 0:128], AF.Exp, [b_ph], [b_ek], scale=-1.0)
                        if need_q:
                            kb.act(eq[:], ph[:, 128:256], AF.Exp, [b_ph], [b_eq])
                        c0 = 63 if d == 0 else 0
                        kb.act(dec[:, i, d, :, h], ph[:, c0:128:64], AF.Exp, [b_ph], [b_dec])
                        kb.tt("dve", kqst[:, d, h, 0, :], kq[:, h, cs], ek[:], ALU.mult, [b_kq, b_ek], [b_kqst])
                        if need_q:
                            kb.tt("dve", kqst[:, d, h, 1, :], kq[:, 4 + h, cs], eq[:], ALU.mult, [b_kq, b_eq], [b_kqst])
                kb.dma(S["KQT"][i], kqst[:], reads=[b_kqst], writes=[db("KQT")])
        kb.P.barrier()


def phase_g(kb, I, S, l, C):
    db = kb.db
    identb, b_identb = C["identb"]
    amf, b_amf = C["amf"]
    amb, b_amb = C["amb"]
    dec, b_dec = C["dec"]
    gnw, b_gnw = C["gnw"]
    am = [(amf, b_amf), (amb, b_amb)]
    with ExitStack() as es:
        sstk = [kb.sb(es, "sstk%d" % d, [128, NTILE, 4, 128], BF16) for d in range(2)]
        srun = [kb.sb(es, "srun%d" % d, [128, 4, 128], F32) for d in range(2)]
        tmpr = kb.sbr(es, "tmpS", [128, 4, 128], F32, 2)
        kdr = kb.sbr(es, "kdt", [128, 512], BF16, 3)
        vr = kb.sbr(es, "vt", [128, 512], BF16, 3)
        kqr = kb.sbr(es, "kqt", [128, 2, 4, 2, 128], BF16, 2)
        grr = kb.sbr(es, "grt", [128, 512], BF16, 2)
        attr = kb.sbr(es, "attm", [128, 128], BF16, 4)
        junk, b_junk = kb.sb(es, "junkg", [128, 128], F32)
        ssqr = kb.sbr(es, "ssq", [128, 8], F32, 2)
        onr = kb.sbr(es, "on", [128, 4, 128], F32, 2)
        ogr = kb.sbr(es, "og", [128, 512], BF16, 2)
        ogTr = kb.sbr(es, "ogT", [128, 4, 128], BF16, 2)
        pP = kb.psr(es, "pP", [128, 512], 2)
        patt = kb.psr(es, "patt", [128, 512], 2)
        po_r = kb.psr(es, "po", [128, 512], 2)
        ptb = kb.psr(es, "ptb", [128, 4, 128], 2, dt=BF16)

        for d in range(2):
            kb.memset("dve", srun[d][0][:], 0.0, [srun[d][1]])
        own_hi = 2 + NOWN // 128
        orders = [list(range(own_hi if l == 1 else NTILE)), [1, 0] + list(range(NTILE - 1, 1, -1))]
        for step in range(NTILE):
            for d in range(2):
                if step >= len(orders[d]):
                    continue
                i = orders[d][step]
                kd, b_kd = kdr.next()
                v, b_v = vr.next()
                kb.dma(kd[:], S["KD"][i, d], reads=[db("KD")], writes=[b_kd])
                kb.dma(v[:], S["V"][i * 128:(i + 1) * 128, :], reads=[db("V")], writes=[b_v])
                sr, b_sr = srun[d]
                st, b_st = sstk[d]
                for c in ((0, 1) if d == 0 else (1, 0)):
                    hs = slice(c * 64, (c + 1) * 64)
                    kb.cp("act", st[hs, i, :, :], sr[hs, :, :], [b_sr], [b_st])
                    pp, b_pp = pP.next()
                    for h in range(4):
                        fs = slice(h * 128, (h + 1) * 128)
                        kb.mm(pp[:, fs], kd[hs, fs], v[hs, fs], True, True, [b_kd, b_v], [b_pp])
                    tmp, b_tmp = tmpr.next()
                    kb.tt("dve", tmp[:], pp[:].rearrange("p (h e) -> p h e", h=4), sr[:], ALU.add, [b_pp, b_sr], [b_tmp])
                    kb.tt("dve", sr[:], tmp[:], dec[:, i, d, c, :].unsqueeze(2).to_broadcast([128, 4, 128]), ALU.mult,
                          [b_tmp, b_dec], [b_sr])
        kb.dump("srun0", srun[0][0][:], [srun[0][1]])
        kb.dump("sstk0", sstk[0][0][:], [sstk[0][1]])

        for i in range(NTILE):
            if l == 1 and (i < 2 or i >= own_hi):
                continue
            kqt, b_kqt = kqr.next()
            v, b_v = vr.next()
            gr, b_gr = grr.next()
            kb.dma(kqt[:], S["KQT"][i], reads=[db("KQT")], writes=[b_kqt])
            kb.dma(v[:], S["V"][i * 128:(i + 1) * 128, :], reads=[db("V")], writes=[b_v])
            kb.dma(gr[:], S["GR"][i * 128:(i + 1) * 128, :], reads=[db("GR")], writes=[b_gr])
            po, b_po = po_r.next()
            for h in range(4):
                fs = slice(h * 128, (h + 1) * 128)
                atts = []
                for d in range(2):
                    slot = (h * 2 + d) % 4
                    if slot == 0:
                        pa, b_pa = patt.next()
                    kb.mm(pa[:, slot * 128:(slot + 1) * 128], kqt[:, d, h, 0, :], kqt[:, d, h, 1, :], True, True,
                          [b_kqt], [b_pa])
                    at, b_at = attr.next()
                    kb.tt("dve", at[:], pa[:, slot * 128:(slot + 1) * 128], am[d][0][:], ALU.mult, [b_pa, am[d][1]], [b_at])
                    atts.append((at, b_at))
                kb.mm(po[:, fs], atts[0][0][:], v[:, fs], True, False, [atts[0][1], b_v], [b_po])
                kb.mm(po[:, fs], kqt[:, 0, h, 1, :], sstk[0][0][:, i, h, :], False, False, [b_kqt, sstk[0][1]], [b_po])
                kb.mm(po[:, fs], atts[1][0][:], v[:, fs], False, False, [atts[1][1], b_v], [b_po])
                kb.mm(po[:, fs], kqt[:, 1, h, 1, :], sstk[1][0][:, i, h, :], False, True, [b_kqt, sstk[1][1]], [b_po])
            ssq, b_ssq = ssqr.next()
            for h in range(4):
                kb.act(junk[:], po[:, h * 128:(h + 1) * 128], AF.Square, [b_po], [b_junk, b_ssq],
                       scale=1.0 / math.sqrt(128.0), accum_out=ssq[:, h:h + 1])
            kb.act(ssq[:, 4:8], ssq[:, 0:4], AF.Sqrt, [b_ssq], [b_ssq], bias=EPS, scale=1.0)
            kb.P.op("dve", lambda E, ssq=ssq: E.reciprocal(out=ssq[:, 4:8], in_=ssq[:, 4:8]), [b_ssq], [b_ssq])
            on, b_on = onr.next()
            kb.tt("dve", on[:], po[:].rearrange("p (h e) -> p h e", h=4),
                  ssq[:, 4:8].unsqueeze(2).to_broadcast([128, 4, 128]), ALU.mult, [b_po, b_ssq], [b_on])
            og, b_og = ogr.next()
            kb.tt("pool", og[:], on[:].rearrange("p h e -> p (h e)"), gr[:], ALU.mult, [b_on, b_gr], [b_og])
            pt, b_pt = ptb.next()
            for h in range(4):
                kb.tr(pt[:, h, :], og[:, h * 128:(h + 1) * 128], identb[:], [b_og, b_identb], [b_pt])
            ogT, b_ogT = ogTr.next()
            kb.act(ogT[:], pt[:], AF.Copy, [b_pt, b_gnw], [b_ogT], scale=gnw[:, 0:1])
            kb.dma(S["OGT"].rearrange("(h e) t -> e h t", h=4)[:, :, i * 128:(i + 1) * 128], ogT[:], reads=[b_ogT],
                   writes=[db("OGT")])
        kb.P.barrier()


MAGIC = 12582912.0
TWO_PI = 2.0 * math.pi
COL_ORDER = [list(range(68)), [3, 2, 1, 0] + list(range(67, 3, -1))]


def phase_s5(kb, I, S, l, C, last=False):
    db = kb.db
    nc = kb.nc
    identf, b_identf = C["identf"]
    identb, b_identb = C["identb"]
    with ExitStack() as es:
        aT, b_aT = kb.sb(es, "aT", [64, 4, 32], F32)
        cT, b_cT = kb.sb(es, "cT", [64, 2, 512], F32)
        dtt, b_dt = kb.sb(es, "dtt", [64, 2, 32], F32)
        ardt, b_ardt = kb.sb(es, "ardt", [64, 2, 32], F32)
        aidt, b_aidt = kb.sb(es, "aidt", [64, 2, 32], F32)
        Bri, b_Bri = kb.sb(es, "Bri", [64, 2, 512], F32)
        Bb, b_Bb = kb.sb(es, "Bb", [64, 2, 2, 512], F32)
        nv, b_nv = kb.sb(es, "nv", [64, 2, NVEC], F32)
        dcol, b_dcol = kb.sb(es, "dcol", [128, 32], F32)
        HBbd = [kb.sb(es, "HBb%d" % d, [64, 2, 32, 68], BF16) for d in range(2)]
        yctx, b_yctx = kb.sb(es, "yctx", [128, 32, 8, 4], F32)
        ur = kb.sbr(es, "ug", [128, 8, 68], BF16, 3)
        mge, b_mge = kb.sb(es, "mge", [128, 128], F32)
        mle, b_mle = kb.sb(es, "mle", [128, 128], F32)

        def load_u(g):
            u, b_u = ur.next()
            kb.ugid = getattr(kb, "ugid", 0) + 1
            for sl in range(8):
                src = bass.AP(tensor=S["UTL"].tensor, offset=g * 16 * NLAT + sl * 64, ap=[[NLAT, 16], [512, 8], [1, 64]])
                kb.dma(u[sl * 16:(sl + 1) * 16, :, 4:68], src, reads=[db("UTL")], writes=[b_u], gid=("u", kb.ugid))
                src = bass.AP(tensor=S["UTC"].tensor, offset=g * 16 * NCTX + sl * 32, ap=[[NCTX, 16], [4, 8], [1, 4]])
                kb.dma(u[sl * 16:(sl + 1) * 16, :, 0:4], src, reads=[db("UTC")], writes=[b_u], gid=("u", kb.ugid))
            return u, b_u

        with ExitStack() as es1:
            rows, b_rows = kb.sb(es1, "rows", [128, 9, 64], F32)
            ptr, b_ptr = kb.ps(es1, "ptrs", [64, 512])
            for w_, nm in enumerate(["s5_a_re_f", "s5_a_im_f", "s5_a_re_b", "s5_a_im_b"]):
                kb.dma(rows[w_ * 32:(w_ + 1) * 32, 0, :], I[nm][l], writes=[b_rows])
            for ri, nm in enumerate(["s5_c_re", "s5_c_im"]):
                kb.dma(rows[:, 1 + 4 * ri:5 + 4 * ri, :], I[nm][l].rearrange("(t g) h p -> (g h) t p", t=4), writes=[b_rows])
            kb.tr(ptr[:, 0:128], rows[:, 0, :], identf[:], [b_rows, b_identf], [b_ptr])
            kb.cp("dve", aT[:].rearrange("p w g -> p (w g)"), ptr[:, 0:128], [b_ptr], [b_aT])
            for ri in range(2):
                for t in range(4):
                    kb.tr(ptr[:, t * 128:(t + 1) * 128], rows[:, 1 + 4 * ri + t, :], identf[:], [b_rows, b_identf], [b_ptr])
                kb.cp("dve", cT[:, ri, :], ptr[:, 0:512], [b_ptr], [b_cT])
            for d, nm in enumerate(["s5_log_dt_f", "s5_log_dt_b"]):
                src = bass.AP(tensor=I[nm].tensor, offset=l * 32, ap=[[0, 64], [1, 32]])
                kb.dma(dtt[:, d, :], src, writes=[b_dt])
            kb.act(dtt[:], dtt[:], AF.Exp, [b_dt], [b_dt])
            for d in range(2):
                kb.tt("dve", ardt[:, d, :], aT[:, 2 * d, :], dtt[:, d, :], ALU.mult, [b_aT, b_dt], [b_ardt])
                kb.tt("dve", aidt[:, d, :], aT[:, 2 * d + 1, :], dtt[:, d, :], ALU.mult, [b_aT, b_dt], [b_aidt])
            kb.dma(Bri[:, 0, :].rearrange("p (g h) -> p g h", h=16), I["s5_b_re"][l].rearrange("g p h -> p g h"), writes=[b_Bri])
            kb.dma(Bri[:, 1, :].rearrange("p (g h) -> p g h", h=16), I["s5_b_im"][l].rearrange("g p h -> p g h"), writes=[b_Bri])
            kb.dma(nv[:].rearrange("p d n -> p (d n)"),
                   bass.AP(tensor=I["k_nvec"].tensor, offset=0, ap=[[0, 64], [1, 2 * NVEC]]), writes=[b_nv])
            for sl in range(8):
                kb.dma(dcol[sl * 16:(sl + 1) * 16, :], I["s5_d"][l].rearrange("(g h) -> h g", h=16), writes=[b_dcol],
                       gid=("dcol", l), allow_slow_non_contiguous=True)
            kb.dma(mge[:], I["k_mge"], writes=[b_mge])
            kb.dma(mle[:], I["k_mle"], writes=[b_mle])
            kb.P.barrier()

        def pw_tables(es_, lo, hi, tag):
            n = hi - lo
            PW, b_PW = kb.sb(es_, "PW" + tag, [64, 2, 2, 32, n], F32)
            with ExitStack() as e2:
                E_, b_E = kb.sb(e2, "pwE", [64, 32, n], F32)
                th, b_th = kb.sb(e2, "pwT", [64, 32, n], F32)
                ar, b_ar = kb.sb(e2, "pwA", [64, 32, n], F32)
                kk, b_kk = kb.sb(e2, "pwK", [64, 32, n], F32)
                for d in range(2):
                    nvb = nv[:, d, lo:hi].unsqueeze(1).to_broadcast([64, 32, n])
                    kb.tt("dve", E_[:], ardt[:, d, :].unsqueeze(2).to_broadcast([64, 32, n]), nvb, ALU.mult,
                          [b_ardt, b_nv], [b_E])
                    kb.act(E_[:], E_[:], AF.Exp, [b_E], [b_E])
                    kb.tt("dve", th[:], aidt[:, d, :].unsqueeze(2).to_broadcast([64, 32, n]), nvb, ALU.mult,
                          [b_aidt, b_nv], [b_th])
                    for ri, ph in ((0, 0.5 * math.pi), (1, 0.0)):
                        kb.ts("dve", ar[:], th[:], ph, None, ALU.add, None, [b_th], [b_ar])
                        kb.ts("dve", kk[:], ar[:], 1.0 / TWO_PI, MAGIC, ALU.mult, ALU.add, [b_ar], [b_kk])
                        kb.ts("dve", kk[:], kk[:], -MAGIC, None, ALU.add, None, [b_kk], [b_kk])
                        kb.stt("dve", ar[:], kk[:], -TWO_PI, ar[:], ALU.mult, ALU.add, [b_kk, b_ar], [b_ar])
                        kb.act(ar[:], ar[:], AF.Sin, [b_ar], [b_ar])
                        kb.tt("dve", PW[:, d, ri, :, :], ar[:], E_[:], ALU.mult, [b_ar, b_E], [b_PW])
                kb.P.barrier()
            return PW, b_PW

        def cmul(eng, out_re, out_im, a_re, a_im, b_re, b_im, t1, t2, reads, w_re, w_im, b_t, neg_im=False):
            kb.tt(eng, t1, a_re, b_re, ALU.mult, reads, [b_t])
            kb.tt(eng, t2, a_im, b_im, ALU.mult, reads, [b_t])
            kb.tt(eng, out_re, t1, t2, ALU.subtract, [b_t], [w_re])
            kb.tt(eng, t1, a_re, b_im, ALU.mult, reads, [b_t])
            kb.tt(eng, t2, a_im, b_re, ALU.mult, reads, [b_t])
            if neg_im:
                kb.tt(eng, t1, t1, t2, ALU.add, [b_t], [b_t])
                kb.ts(eng, out_im, t1, -1.0, None, ALU.mult, None, [b_t], [w_im])
            else:
                kb.tt(eng, out_im, t1, t2, ALU.add, [b_t], [w_im])

        s5stop = getattr(kb, "s5stop", 9)
        if s5stop <= 1:
            kb.dump("aT", aT[:], [b_aT]); kb.dump("cT", cT[:], [b_cT]); kb.dump("dtt", dtt[:], [b_dt]); kb.dump("nv", nv[:], [b_nv])
            kb.dump("dcol", dcol[:], [b_dcol]); kb.dump("Bri", Bri[:], [b_Bri])
            kb.P.barrier()
            return
        with ExitStack() as es1:
            PWq = []
            for d, idx in ((0, 72 + 62), (1, 72 + 1)):
                PWq.append(pw_tables(es1, idx, idx + 1, "q%d" % d))
            q, b_q = kb.sb(es1, "q", [64, 8, 32], F32)
            t1, b_t = kb.sb(es1, "qt", [64, 2, 512], F32)
            for d in range(2):
                PWd, b_PWd = PWq[d]
                lbr = PWd[:, d, 0, :, 0]
                lbi = PWd[:, d, 1, :, 0]
                are, aim = aT[:, 2 * d, :], aT[:, 2 * d + 1, :]
                kb.ts("dve", q[:, 0, :], lbr, -1.0, None, ALU.add, None, [b_PWd], [b_q])
                kb.tt("dve", q[:, 1, :], are, are, ALU.mult, [b_aT], [b_q])
                kb.tt("dve", q[:, 2, :], aim, aim, ALU.mult, [b_aT], [b_q])
                kb.tt("dve", q[:, 1, :], q[:, 1, :], q[:, 2, :], ALU.add, [b_q], [b_q])
                kb.P.op("dve", lambda E, q=q: E.reciprocal(out=q[:, 1, :], in_=q[:, 1, :]), [b_q], [b_q])
                kb.tt("dve", q[:, 2, :], q[:, 0, :], are, ALU.mult, [b_q, b_aT], [b_q])
                kb.tt("dve", q[:, 3, :], lbi, aim, ALU.mult, [b_PWd, b_aT], [b_q])
                kb.tt("dve", q[:, 2, :], q[:, 2, :], q[:, 3, :], ALU.add, [b_q], [b_q])
                kb.tt("dve", q[:, 4, :], q[:, 2, :], q[:, 1, :], ALU.mult, [b_q], [b_q])
                kb.tt("dve", q[:, 2, :], lbi, are, ALU.mult, [b_PWd, b_aT], [b_q])
                kb.tt("dve", q[:, 3, :], q[:, 0, :], aim, ALU.mult, [b_q, b_aT], [b_q])
                kb.tt("dve", q[:, 2, :], q[:, 2, :], q[:, 3, :], ALU.subtract, [b_q], [b_q])
                kb.tt("dve", q[:, 5, :], q[:, 2, :], q[:, 1, :], ALU.mult, [b_q], [b_q])
                qre = q[:, 4, :].unsqueeze(2).to_broadcast([64, 32, 16])
                qim = q[:, 5, :].unsqueeze(2).to_broadcast([64, 32, 16])
                v3 = lambda ap: ap.rearrange("p (g h) -> p g h", h=16)
                cmul("dve", v3(Bb[:, d, 0, :]), v3(Bb[:, d, 1, :]), qre, qim, v3(Bri[:, 0, :]), v3(Bri[:, 1, :]),
                     v3(t1[:, 0, :]), v3(t1[:, 1, :]), [b_q, b_Bri], b_Bb, b_Bb, b_t)
            kb.P.barrier()

        if s5stop <= 2:
            kb.dump("Bb", Bb[:], [b_Bb])
            kb.P.barrier()
            return
        with ExitStack() as es1:
            PW1, b_PW1 = pw_tables(es1, 72, 137, "1")
            HL, b_HL = kb.sb(es1, "HL", [64, 2, 2, 32, 68], F32)
            with ExitStack() as es2:
                Xr = [kb.sbr(es2, "X%d" % d, [64, 2, 1024], BF16, 2) for d in range(2)]
                tr_ = [kb.sbr(es2, "xt12%d" % d, [64, 2, 1024], F32, 2) for d in range(2)]
                SIr = kb.sbr(es2, "SI", [128, 2, 2, 8, 64], BF16, 2)
                ptx = kb.psr(es2, "ptx", [128, 2048], 2, dt=BF16)
                phl = kb.psr(es2, "phl", [64, 4, 128], 2)
                def gen1(g):
                    Xd = [Xr[d].next() for d in range(2)]
                    for d in range(2):
                        X, b_X = Xd[d]
                        t12, b_t12 = tr_[d].next()
                        v3 = lambda ap: ap.rearrange("p (s h) -> p s h", h=16)
                        pre = PW1[:, d, 0, g, 0:64].unsqueeze(2).to_broadcast([64, 64, 16])
                        pim = PW1[:, d, 1, g, 0:64].unsqueeze(2).to_broadcast([64, 64, 16])
                        bre = Bb[:, d, 0, g * 16:(g + 1) * 16].unsqueeze(1).to_broadcast([64, 64, 16])
                        bim = Bb[:, d, 1, g * 16:(g + 1) * 16].unsqueeze(1).to_broadcast([64, 64, 16])
                        cmul("dve", v3(X[:, 0, :]), v3(X[:, 1, :]), pre, pim, bre, bim, v3(t12[:, 0, :]), v3(t12[:, 1, :]),
                             [b_PW1, b_Bb], b_X, b_X, b_t12)
                    return Xd

                def use1(g, Xd):
                    u, b_u = load_u(g)
                    px, b_px = ptx.next()
                    SI, b_SI = SIr.next()
                    for d in range(2):
                        for ri in range(2):
                            for sh in range(8):
                                o_ = ((d * 2 + ri) * 8 + sh) * 64
                                kb.tr(px[:, o_:o_ + 64], Xd[d][0][:, ri, sh * 128:(sh + 1) * 128], identb[0:64, 0:64],
                                      [Xd[d][1], b_identb], [b_px])
                    for q4 in range(4):
                        kb.cp("act", SI[:].rearrange("p d r s c -> p (d r s c)")[:, q4 * 512:(q4 + 1) * 512],
                              px[:, q4 * 512:(q4 + 1) * 512], [b_px], [b_SI])
                    ph, b_ph = phl.next()
                    for d in range(2):
                        for ri in range(2):
                            for sh in range(8):
                                kb.mm(ph[:, d * 2 + ri, 0:68], SI[:, d, ri, sh, :], u[:, sh, :], sh == 0, sh == 7,
                                      [b_SI, b_u], [b_ph])
                    kb.cp("act", HL[:, :, :, g, :].rearrange("p d r c -> p (d r) c"), ph[:, :, 0:68], [b_ph], [b_HL])

                Xn = gen1(0)
                for g in range(32):
                    Xc = Xn
                    if g + 1 < 32:
                        Xn = gen1(g + 1)
                    use1(g, Xc)
                kb.P.barrier()
            if s5stop <= 3:
                kb.dump("HL", HL[:], [b_HL])
                kb.P.barrier()
                return
            with ExitStack() as es2:
                st = [kb.sb(es2, "sct%d" % d, [64, 4, 32], F32) for d in range(2)]
                sc = [kb.sb(es2, "scs%d" % d, [64, 2, 2, 32], F32) for d in range(2)]
                for d in range(2):
                    eng = "dve" if d == 0 else "pool"
                    kb.memset(eng, sc[d][0][:], 0.0, [sc[d][1]])
                    kb.memset(eng, HBbd[d][0][:, :, :, COL_ORDER[d][0]], 0.0, [HBbd[d][1]])
                for j in range(67):
                    for d in range(2):
                        eng = "dve" if d == 0 else "pool"
                        t, b_t = st[d]
                        s_, b_s = sc[d]
                        cur, nxt = COL_ORDER[d][j], COL_ORDER[d][j + 1]
                        pi, po = j % 2, (j + 1) % 2
                        lre, lim = PW1[:, d, 0, :, 64], PW1[:, d, 1, :, 64]
                        hre, him = s_[:, pi, 0, :], s_[:, pi, 1, :]
                        kb.tt(eng, t[:, 0, :], lre, hre, ALU.mult, [b_PW1, b_s], [b_t])
                        kb.tt(eng, t[:, 1, :], lim, him, ALU.mult, [b_PW1, b_s], [b_t])
                        kb.tt(eng, t[:, 0, :], t[:, 0, :], t[:, 1, :], ALU.subtract, [b_t], [b_t])
                        kb.tt(eng, s_[:, po, 0, :], t[:, 0, :], HL[:, d, 0, :, cur], ALU.add, [b_t, b_HL], [b_s])
                        kb.tt(eng, t[:, 2, :], lre, him, ALU.mult, [b_PW1, b_s], [b_t])
                        kb.tt(eng, t[:, 3, :], lim, hre, ALU.mult, [b_PW1, b_s], [b_t])
                        kb.tt(eng, t[:, 2, :], t[:, 2, :], t[:, 3, :], ALU.add, [b_t], [b_t])
                        kb.tt(eng, s_[:, po, 1, :], t[:, 2, :], HL[:, d, 1, :, cur], ALU.add, [b_t, b_HL], [b_s])
                        kb.cp(eng, HBbd[d][0][:, 0, :, nxt], s_[:, po, 0, :], [b_s], [HBbd[d][1]])
                        kb.ts(eng, HBbd[d][0][:, 1, :, nxt], s_[:, po, 1, :], -1.0, None, ALU.mult, None, [b_s], [HBbd[d][1]])
                kb.P.barrier()

        if s5stop <= 4:
            kb.P.barrier()
            return
        with ExitStack() as es1:
            PW2, b_PW2 = pw_tables(es1, 0, 72, "2")
            with ExitStack() as es2:
                CLr = [kb.sbr(es2, "CL%d" % d, [64, 2, 1024], BF16, 2) for d in range(2)]
                RSr = [kb.sbr(es2, "RS%d" % d, [64, 2, 128], BF16, 2) for d in range(2)]
                t2r = [kb.sbr(es2, "t2%d" % d, [64, 2, 1024], F32, 2) for d in range(2)]
                TBr = kb.sbr(es2, "TB", [128, 2, 8, 128], BF16, 2)
                t0r = kb.sbr(es2, "t0", [128, 2, 128], F32, 2)
                ysr = kb.sbr(es2, "ys", [128, 8, 68], F32, 2)
                pT = kb.psr(es2, "pT", [128, 2048], 1)
                pY = kb.psr(es2, "pY", [128, 8, 128], 2)
                NTH = 4 if last else 8

                def gen2(g):
                    CLd = [CLr[d].next() for d in range(2)]
                    RSd = [RSr[d].next() for d in range(2)]
                    for d in range(2):
                        CL, b_CL = CLd[d]
                        RS, b_RS = RSd[d]
                        t2, b_t2 = t2r[d].next()
                        v3 = lambda ap: ap.rearrange("p (s h) -> p s h", h=16)
                        pre = PW2[:, d, 0, g, 0:64].unsqueeze(2).to_broadcast([64, 64, 16])
                        pim = PW2[:, d, 1, g, 0:64].unsqueeze(2).to_broadcast([64, 64, 16])
                        cre = cT[:, 0, g * 16:(g + 1) * 16].unsqueeze(1).to_broadcast([64, 64, 16])
                        cim = cT[:, 1, g * 16:(g + 1) * 16].unsqueeze(1).to_broadcast([64, 64, 16])
                        cmul("dve", v3(CL[:, 0, :]), v3(CL[:, 1, :]), pre, pim, cre, cim, v3(t2[:, 0, :]), v3(t2[:, 1, :]),
                             [b_PW2, b_cT], b_CL, b_CL, b_t2)
                        pre = PW2[:, d, 0, g, 64:72].unsqueeze(2).to_broadcast([64, 8, 16])
                        pim = PW2[:, d, 1, g, 64:72].unsqueeze(2).to_broadcast([64, 8, 16])
                        bre = Bb[:, d, 0, g * 16:(g + 1) * 16].unsqueeze(1).to_broadcast([64, 8, 16])
                        bim = Bb[:, d, 1, g * 16:(g + 1) * 16].unsqueeze(1).to_broadcast([64, 8, 16])
                        cmul("dve", v3(RS[:, 0, :]), v3(RS[:, 1, :]), pre, pim, bre, bim, v3(t2[:, 0, 0:128]),
                             v3(t2[:, 1, 0:128]), [b_PW2, b_Bb], b_RS, b_RS, b_t2, neg_im=True)
                    return CLd, RSd

                def use2(g, CLd, RSd):
                    u, b_u = load_u(g)
                    pt, b_pt = pT.next()
                    for d in range(2):
                        for dl in range(8):
                            o_ = (d * 8 + dl) * 128
                            kb.mm(pt[:, o_:o_ + 128], RSd[d][0][:, 0, :], CLd[d][0][:, 0, dl * 128:(dl + 1) * 128], True, False,
                                  [RSd[d][1], CLd[d][1]], [b_pt])
                            kb.mm(pt[:, o_:o_ + 128], RSd[d][0][:, 1, :], CLd[d][0][:, 1, dl * 128:(dl + 1) * 128], False, True,
                                  [RSd[d][1], CLd[d][1]], [b_pt])
                    TB, b_TB = TBr.next()
                    for q4 in range(4):
                        kb.cp("act", TB[:].rearrange("p d s c -> p (d s c)")[:, q4 * 512:(q4 + 1) * 512],
                              pt[:, q4 * 512:(q4 + 1) * 512], [b_pt], [b_TB])
                    t0, b_t0 = t0r.next()
                    kb.tt("dve", t0[:, 0, :], pt[:, 0:128], mge[:], ALU.mult, [b_pt, b_mge, b_TB], [b_t0])
                    kb.tt("dve", t0[:, 1, :], pt[:, 1024:1152], mle[:], ALU.mult, [b_pt, b_mle, b_TB], [b_t0])
                    kb.tt("dve", t0[:, 0, :], t0[:, 0, :], t0[:, 1, :], ALU.add, [b_t0], [b_t0])
                    kb.stt("dve", TB[:, 0, 0, :], identf[:], dcol[:, g:g + 1], t0[:, 0, :], ALU.mult, ALU.add,
                           [b_identf, b_dcol, b_t0], [b_TB])
                    py, b_py = pY.next()
                    for th_ in range(NTH):
                        for sh in range(8):
                            dl = th_ - sh
                            lhsT = TB[:, 0, dl, :] if dl >= 0 else TB[:, 1, -dl, :]
                            kb.mm(py[:, th_, 0:68], lhsT, u[:, sh, :], sh == 0, False, [b_TB, b_u], [b_py])
                        jf = th_ * 128
                        jb = (7 - th_) * 128
                        kb.mm(py[:, th_, 0:68], CLd[0][0][:, 0, jf:jf + 128], HBbd[0][0][:, 0, g, :], False, False,
                              [CLd[0][1], HBbd[0][1]], [b_py])
                        kb.mm(py[:, th_, 0:68], CLd[0][0][:, 1, jf:jf + 128], HBbd[0][0][:, 1, g, :], False, False,
                              [CLd[0][1], HBbd[0][1]], [b_py])
                        kb.mm(py[:, th_, 0:68], CLd[1][0][:, 0, jb:jb + 128], HBbd[1][0][:, 0, g, :], False, False,
                              [CLd[1][1], HBbd[1][1]], [b_py])
                        kb.mm(py[:, th_, 0:68], CLd[1][0][:, 1, jb:jb + 128], HBbd[1][0][:, 1, g, :], False, True,
                              [CLd[1][1], HBbd[1][1]], [b_py])
                    ys, b_ys = ysr.next()
                    kb.cp("act", ys[:, 0:4, :], py[:, 0:4, 0:68], [b_py], [b_ys])
                    if not last:
                        kb.cp("act", ys[:, 4:8, :], py[:, 4:8, 0:68], [b_py], [b_ys])
                    for tl in range(8):
                        dst = bass.AP(tensor=S["YTL"].tensor, offset=g * 16 * NLAT + tl * 64, ap=[[NLAT, 16], [512, NTH], [1, 64]])
                        kb.dma(dst, ys[tl * 16:(tl + 1) * 16, 0:NTH, 4:68], reads=[b_ys], writes=[db("YTL")])
                    if not last:
                        kb.cp("pool", yctx[:, g, :, :], ys[:, :, 0:4], [b_ys], [b_yctx])

                gn = gen2(0)
                for g in range(32):
                    gc = gn
                    if g + 1 < 32:
                        gn = gen2(g + 1)
                    use2(g, *gc)
                for tl in range(8 if (s5stop > 6 and not last) else 0):
                    dst = bass.AP(tensor=S["YTC"].tensor, offset=tl * 32, ap=[[NCTX, 16], [16 * NCTX, 32], [1, 32]])
                    kb.dma(dst, yctx[tl * 16:(tl + 1) * 16, :, :, :].rearrange("p g a c -> p g (a c)"), reads=[b_yctx],
                           writes=[db("YTC")])
                kb.P.barrier()
            kb.P.barrier()
        kb.P.barrier()


def phase_m(kb, I, S, l, C, last):
    db = kb.db
    identf, b_identf = C["identf"]
    vpt, b_vpt = C["vpt"]
    ab, b_ab = C["ab"]
    gbc, b_gbc = C["gbc"]
    gates, b_gates = C["gates"]
    with ExitStack() as es:
        wgp, b_wgp = kb.sb(es, "wgp", [128, 4, D], BF16)
        wglu, b_wglu = kb.sb(es, "wglu", [128, 4, 512], BF16)
        ws5, b_ws5 = kb.sb(es, "ws5", [128, 4, D], BF16)
        wout, b_wout = kb.sb(es, "wout", [128, KT, D], BF16)
        kb.dma(wgp[:], I["w_gla_proj"][l].rearrange("(k p) n -> p k n", p=128), writes=[b_wgp], q="pool")
        kb.dma(wglu[:], I["s5_w_glu"][l].rearrange("(k p) n -> p k n", p=128), writes=[b_wglu], q="pool")
        kb.dma(ws5[:], I["w_s5_proj"][l].rearrange("(k p) n -> p k n", p=128), writes=[b_ws5], q="pool")
        for k in range(KT):
            kb.dma(wout[:, k, :], I["w_out"][l][k * 128:(k + 1) * 128, :], writes=[b_wout], q="pool", gid=("wout", l))
        if last:
            wr, b_wr = kb.sb(es, "wr", [128, KT, NEXP], F32)
            kb.dma(wr[:], I["moe_router"][0].rearrange("(k p) e -> p k e", p=128), writes=[b_wr])
        ogr = kb.sbr(es, "ogt", [128, 4, 512], BF16, 1)
        ytr = kb.sbr(es, "yt", [128, 4, 512], F32, 1)
        gar = kb.sbr(es, "gat", [128, 8, 512], BF16, 1)
        gmr = kb.sbr(es, "gmt", [128, 8, 512], BF16, 1)
        gt, b_gt = kb.sb(es, "gelt", [128, 4, 512], F32)
        sgm, b_sgm = kb.sb(es, "gelsg", [128, 4, 512], F32)
        s1, b_s1 = kb.sb(es, "s1", [128, 4, 512], BF16)
        s2, b_s2 = kb.sb(es, "s2", [128, 4, 512], BF16)
        sigr = kb.sbr(es, "sig", [128, 512], F32, 2)
        mT, b_mT = kb.sb(es, "mT", [128, 8, 512], BF16)
        t1r = kb.sbr(es, "t1", [128, 512], F32, 2)
        t2r = kb.sbr(es, "t2m", [128, 512], F32, 2)
        ltr = kb.sbr(es, "lt", [128, D], F32, 2)
        tmpr = kb.sbr(es, "tmpm", [128, D], F32, 1)
        l2r = kb.sbr(es, "lat2", [128, D], F32, 2)
        xs2r = kb.sbr(es, "xs2", [128, D], F32, 1)
        ssr = kb.sbr(es, "ssm", [128, 2], F32, 4)
        h2fr = kb.sbr(es, "h2f", [128, KT, 128], F32, 2)
        h2br = kb.sbr(es, "h2b", [128, KT, 128], BF16, 2)
        lgr = kb.sbr(es, "lg", [128, 8, 8], F32, 2)
        pab = kb.psr(es, "pab", [128, 512], 3)
        pml = kb.psr(es, "pml", [128, D], 1)
        pt2 = kb.psr(es, "pt2", [128, KT, 128], 1)
        plg = kb.psr(es, "plg", [128, 8], 1)

        if last:
            groups = [(2 + 4 * g, 4) for g in range(NOWN // 512)]
        else:
            groups = [(0, 2)] + [(2 + 4 * g, 4) for g in range(8)]
        for (i0, nt) in groups:
            Ng = 128 * nt
            isctx = i0 == 0
            T0 = i0 * 128
            og, b_og = ogr.next()
            yt, b_yt = ytr.next()
            ga, b_ga = gar.next()
            gm, b_gm = gmr.next()
            kb.dma(og[:, :, 0:Ng], S["OGT"].rearrange("(k p) t -> p k t", p=128)[:, :, T0:T0 + Ng], reads=[db("OGT")], writes=[b_og])
            if isctx:
                kb.dma(yt[:, :, 0:Ng], S["YTC"].rearrange("(k p) t -> p k t", p=128), reads=[db("YTC")], writes=[b_yt])
            else:
                kb.dma(yt[:, :, 0:Ng], S["YTL"].rearrange("(k p) t -> p k t", p=128)[:, :, T0 - NCTX:T0 - NCTX + Ng],
                       reads=[db("YTL")], writes=[b_yt])
            kb.dma(ga[:, :, 0:Ng], S["GAT"].rearrange("(k p) t -> p k t", p=128)[:, :, T0:T0 + Ng], reads=[db("GAT")], writes=[b_ga])
            kb.dma(gm[:, :, 0:Ng], S["GMT"].rearrange("(k p) t -> p k t", p=128)[:, :, T0:T0 + Ng], reads=[db("GMT")], writes=[b_gm])
            y = yt[:, :, 0:Ng]
            kb.tt("dve", gt[:, :, 0:Ng], y, y, ALU.mult, [b_yt], [b_gt])
            kb.ts("dve", gt[:, :, 0:Ng], gt[:, :, 0:Ng], 0.044715, 1.0, ALU.mult, ALU.add, [b_gt], [b_gt])
            kb.tt("dve", gt[:, :, 0:Ng], gt[:, :, 0:Ng], y, ALU.mult, [b_gt, b_yt], [b_gt])
            kb.act(sgm[:, :, 0:Ng], gt[:, :, 0:Ng], AF.Sigmoid, [b_gt], [b_sgm], scale=1.5957691216057308)
            kb.tt("dve", s1[:, :, 0:Ng], y, sgm[:, :, 0:Ng], ALU.mult, [b_yt, b_sgm], [b_s1])
            for of in range(4):
                pz, b_pz = pab.next()
                for k in range(4):
                    kb.mm(pz[:, 0:Ng], wglu[:, k, of * 128:(of + 1) * 128], s1[:, k, 0:Ng], k == 0, k == 3, [b_wglu, b_s1], [b_pz])
                sg, b_sg = sigr.next()
                kb.act(sg[:, 0:Ng], pz[:, 0:Ng], AF.Sigmoid, [b_pz, b_vpt], [b_sg], bias=vpt[:, 64 + of:65 + of], scale=1.0)
                kb.tt("dve", s2[:, of, 0:Ng], s1[:, of, 0:Ng], sg[:, 0:Ng], ALU.mult, [b_s1, b_sg], [b_s2])
            for of in range(8):
                fs = slice(of * 128, (of + 1) * 128)
                pa, b_pa = pab.next()
                for k in range(4):
                    kb.mm(pa[:, 0:Ng], wgp[:, k, fs], og[:, k, 0:Ng], k == 0, k == 3, [b_wgp, b_og], [b_pa])
                pb, b_pb = pab.next()
                for k in range(4):
                    rhs = s2[:, k, 0:Ng]
                    if isctx:
                        rhs = rhs.rearrange("p (b a c) -> p c a b", b=8, a=8, c=4)
                    kb.mm(pb[:, 0:Ng], ws5[:, k, fs], rhs, k == 0, k == 3, [b_ws5, b_s2], [b_pb])
                t1, b_t1 = t1r.next()
                t2, b_t2 = t2r.next()
                kb.tt("dve", t1[:, 0:Ng], pa[:, 0:Ng], ga[:, of, 0:Ng], ALU.mult, [b_pa, b_ga], [b_t1])
                kb.tt("dve", t2[:, 0:Ng], pb[:, 0:Ng], gm[:, of, 0:Ng], ALU.mult, [b_pb, b_gm], [b_t2])
                kb.tt("pool", mT[:, of, 0:Ng], t1[:, 0:Ng], t2[:, 0:Ng], ALU.add, [b_t1, b_t2], [b_mT])
            gi = 2 if isctx else 0
            Ai = 6 if isctx else 4
            for j in range(nt):
                i = i0 + j
                cs = slice(j * 128, (j + 1) * 128)
                pm, b_pm = pml.next()
                for half in range(2):
                    for k in range(KT):
                        kb.mm(pm[:, half * 512:(half + 1) * 512], mT[:, k, cs], wout[:, k, half * 512:(half + 1) * 512],
                              k == 0, k == KT - 1, [b_mT, b_wout], [b_pm])
                lt, b_lt = ltr.next()
                kb.dma(lt[:], S["LAT"][i * 128:(i + 1) * 128, :], reads=[db("LAT", i)], writes=[b_lt])
                tmp, b_tmp = tmpr.next()
                for half in range(2):
                    hs = slice(half * 512, (half + 1) * 512)
                    kb.tt("dve", tmp[:, hs], pm[:, hs], gbc[:, gi, hs], ALU.mult, [b_pm, b_gbc], [b_tmp])
                l2, b_l2 = l2r.next()
                kb.tt("pool", l2[:], tmp[:], lt[:], ALU.add, [b_tmp, b_lt], [b_l2])
                kb.dma(S["LAT"][i * 128:(i + 1) * 128, :], l2[:], reads=[b_l2], writes=[db("LAT", i)])
                xs2, b_xs2 = xs2r.next()
                ss, b_ss = ssr.next()
                kb.act(xs2[:], l2[:], AF.Square, [b_l2], [b_xs2, b_ss], scale=1.0 / 32.0, accum_out=ss[:, 0:1])
                kb.rstd(ss[:, 1:2], ss[:, 0:1], b_ss)
                kb.ts("dve", xs2[:], l2[:], ss[:, 1:2], None, ALU.mult, None, [b_l2, b_ss], [b_xs2])
                pt, b_pt = pt2.next()
                for k in range(KT):
                    kb.tr(pt[:, k, :], xs2[:, k * 128:(k + 1) * 128], identf[:], [b_xs2, b_identf], [b_pt])
                h2f, b_h2f = h2fr.next()
                for k in range(KT):
                    if k < 4:
                        kb.ts("dve", h2f[:, k, :], pt[:, k, :], ab[:, k, Ai:Ai + 1], ab[:, k, Ai + 1:Ai + 2], ALU.mult, ALU.add,
                              [b_pt, b_ab], [b_h2f])
                    else:
                        kb.act(h2f[:, k, :], pt[:, k, :], AF.Identity, [b_pt, b_ab], [b_h2f],
                               scale=ab[:, k, Ai:Ai + 1], bias=ab[:, k, Ai + 1:Ai + 2])
                h2b, b_h2b = h2br.next()
                kb.cp("act", h2b[:], h2f[:], [b_h2f], [b_h2b])
                kb.dma(S["H2T"].rearrange("(k p) t -> p k t", p=128)[:, :, i * 128:(i + 1) * 128], h2b[:], reads=[b_h2b],
                       writes=[db("H2T")])
                if last:
                    pl_, b_pl = plg.next()
                    for k in range(KT):
                        kb.mm(pl_[:, 0:NEXP], h2f[:, k, :], wr[:, k, :], k == 0, k == KT - 1, [b_h2f, b_wr], [b_pl])
                    lg, b_lg = lgr.next()
                    ti = i - 2
                    kb.cp("dve", lg[:, 0, :], pl_[:, 0:NEXP], [b_pl], [b_lg])
                    kb.P.op("dve", lambda E, lg=lg: E.tensor_reduce(out=lg[:, 7, 0:1], in_=lg[:, 0, :], axis=mybir.AxisListType.X,
                                                                    op=ALU.max), [b_lg], [b_lg])
                    kb.ts("dve", lg[:, 1, :], lg[:, 0, :], lg[:, 7, 0:1], None, ALU.is_equal, None, [b_lg], [b_lg])
                    kb.stt("dve", lg[:, 2, :], lg[:, 1, :], -1.0e30, lg[:, 0, :], ALU.mult, ALU.add, [b_lg], [b_lg])
                    kb.P.op("dve", lambda E, lg=lg: E.tensor_reduce(out=lg[:, 7, 1:2], in_=lg[:, 2, :], axis=mybir.AxisListType.X,
                                                                    op=ALU.max), [b_lg], [b_lg])
                    kb.ts("dve", lg[:, 3, :], lg[:, 0, :], lg[:, 7, 1:2], None, ALU.is_ge, None, [b_lg], [b_lg])
                    kb.ts("dve", lg[:, 7, 2:3], lg[:, 7, 0:1], -1.0, None, ALU.mult, None, [b_lg], [b_lg])
                    kb.act(lg[:, 4, :], lg[:, 0, :], AF.Exp, [b_lg], [b_lg], bias=lg[:, 7, 2:3], scale=1.0)
                    kb.tt("dve", lg[:, 5, :], lg[:, 4, :], lg[:, 3, :], ALU.mult, [b_lg], [b_lg])
                    kb.P.op("dve", lambda E, lg=lg: E.tensor_reduce(out=lg[:, 7, 3:4], in_=lg[:, 5, :], axis=mybir.AxisListType.X,
                                                                    op=ALU.add), [b_lg], [b_lg])
                    kb.P.op("dve", lambda E, lg=lg: E.reciprocal(out=lg[:, 7, 4:5], in_=lg[:, 7, 3:4]), [b_lg], [b_lg])
                    kb.ts("dve", gates[:, ti, :], lg[:, 5, :], lg[:, 7, 4:5], None, ALU.mult, None, [b_lg], [b_gates])
        kb.P.barrier()


def phase_f(kb, I, S, OUT, l, C, last):
    db = kb.db
    gbc, b_gbc = C["gbc"]
    gates, b_gates = C["gates"]
    HBK = 256
    if last:
        sgroups = [list(range(2, 2 + NOWN // 128))]
        nexp, Hd = NEXP, H_MOE
    else:
        sgroups = [list(range(0, 17)), list(range(17, 34))]
        nexp, Hd = 1, H_FFN
    nblk = Hd // HBK
    with ExitStack() as es:
        h2, b_h2 = kb.sb(es, "h2", [128, KT, 17 * 128], BF16)
        facc, b_facc = kb.sb(es, "facc", [128, 17, D], F32)
        wgr = kb.sbr(es, "wg", [128, KT, HBK], BF16, 3)
        wur = kb.sbr(es, "wu", [128, KT, HBK], BF16, 3)
        wdr = kb.sbr(es, "wd", [128, HBK // 128, D], BF16, 3)
        sgr = kb.sbr(es, "sgf", [128, 512], F32, 2)
        ar = kb.sbr(es, "aact", [128, HBK // 128, 512], BF16, 3)
        ltr = kb.sbr(es, "ltf", [128, D], F32, 2)
        tmr = kb.sbr(es, "tmf", [128, D], F32, 2)
        otr = kb.sbr(es, "otf", [128, D], F32, 2)
        ssr = kb.sbr(es, "ssf", [128, 2], F32, 4)
        pgu = kb.psr(es, "pgu", [128, 512], 4)
        pdn = kb.psr(es, "pdn", [128, 512], 4)
        for tl in sgroups:
            n = len(tl)
            t0 = tl[0]
            kb.dma(h2[:, :, 0:n * 128], S["H2T"].rearrange("(k p) t -> p k t", p=128)[:, :, t0 * 128:(t0 + n) * 128],
                   reads=[db("H2T")], writes=[b_h2])
            tgs = [list(range(a, min(a + 4, n))) for a in range(0, n, 4)]
            items = []
            for e in range(nexp):
                for blk in range(nblk):
                    for tg in tgs:
                        items.append((e, blk, tg))
            wcur = {}

            def stage_a(item):
                e, blk, tg = item
                if (e, blk) not in wcur:
                    if last:
                        Wg, Wu, Wd = I["moe_w_gate"][0, e], I["moe_w_up"][0, e], I["moe_w_down"][0, e]
                    else:
                        Wg, Wu, Wd = I["ffn_w_gate"][0], I["ffn_w_up"][0], I["ffn_w_down"][0]
                    Wgv = Wg.rearrange("(k p) n -> p k n", p=128)
                    Wuv = Wu.rearrange("(k p) n -> p k n", p=128)
                    Wdv = Wd.rearrange("(c p) n -> p c n", p=128)
                    wg, b_wg = wgr.next()
                    wu, b_wu = wur.next()
                    wd, b_wd = wdr.next()
                    kb.dma(wg[:], Wgv[:, :, blk * HBK:(blk + 1) * HBK], writes=[b_wg], q="pool")
                    kb.dma(wu[:], Wuv[:, :, blk * HBK:(blk + 1) * HBK], writes=[b_wu], q="pool")
                    kb.dma(wd[:], Wdv[:, blk * (HBK // 128):(blk + 1) * (HBK // 128), :], writes=[b_wd], q="pool")
                    wcur.clear()
                    wcur[(e, blk)] = (wg, b_wg, wu, b_wu, wd, b_wd)
                wg, b_wg, wu, b_wu, wd, b_wd = wcur[(e, blk)]
                ntg = len(tg) * 128
                c0 = tg[0] * 128
                a, b_a = ar.next()
                for jc in range(HBK // 128):
                    js = slice(jc * 128, (jc + 1) * 128)
                    pg_, b_pg = pgu.next()
                    for k in range(KT):
                        kb.mm(pg_[:, 0:ntg], wg[:, k, js], h2[:, k, c0:c0 + ntg], k == 0, k == KT - 1, [b_wg, b_h2], [b_pg])
                    pu_, b_pu = pgu.next()
                    for k in range(KT):
                        kb.mm(pu_[:, 0:ntg], wu[:, k, js], h2[:, k, c0:c0 + ntg], k == 0, k == KT - 1, [b_wu, b_h2], [b_pu])
                    sg, b_sg = sgr.next()
                    kb.act(sg[:, 0:ntg], pg_[:, 0:ntg], AF.Silu, [b_pg], [b_sg])
                    kb.tt("dve", a[:, jc, 0:ntg], pu_[:, 0:ntg], sg[:, 0:ntg], ALU.mult, [b_pu, b_sg], [b_a])
                return (a, b_a, wd, b_wd)

            def stage_b(item, st, first):
                e, blk, tg = item
                a, b_a, wd, b_wd = st
                for ti_, t in enumerate(tg):
                    for half in range(2):
                        hs = slice(half * 512, (half + 1) * 512)
                        pd, b_pd = pdn.next()
                        for jc in range(HBK // 128):
                            kb.mm(pd[:], a[:, jc, ti_ * 128:(ti_ + 1) * 128], wd[:, jc, hs], jc == 0, jc == HBK // 128 - 1,
                                  [b_a, b_wd], [b_pd])
                        if last:
                            gw = gates[:, tl[t] - 2, e:e + 1]
                            if first:
                                kb.ts("dve", facc[:, t, hs], pd[:], gw, None, ALU.mult, None, [b_pd, b_gates], [b_facc])
                            else:
                                kb.stt("dve", facc[:, t, hs], pd[:], gw, facc[:, t, hs], ALU.mult, ALU.add,
                                       [b_pd, b_gates, b_facc], [b_facc])
                        else:
                            if first:
                                kb.cp("dve", facc[:, t, hs], pd[:], [b_pd], [b_facc])
                            else:
                                kb.tt("dve", facc[:, t, hs], pd[:], facc[:, t, hs], ALU.add, [b_pd, b_facc], [b_facc])

            stn = stage_a(items[0])
            for ii, item in enumerate(items):
                stc = stn
                if ii + 1 < len(items):
                    stn = stage_a(items[ii + 1])
                stage_b(item, stc, first=(item[0] == 0 and item[1] == 0))
            for t, i in enumerate(tl):
                lt, b_lt = ltr.next()
                kb.dma(lt[:], S["LAT"][i * 128:(i + 1) * 128, :], reads=[db("LAT", i)], writes=[b_lt])
                gi = 3 if i < 2 else 1
                tm, b_tm = tmr.next()
                kb.tt("dve", tm[:], facc[:, t, :], gbc[:, gi, :], ALU.mult, [b_facc, b_gbc], [b_tm])
                kb.tt("pool", tm[:], tm[:], lt[:], ALU.add, [b_tm, b_lt], [b_tm])
                if not last:
                    kb.dma(S["LAT"][i * 128:(i + 1) * 128, :], tm[:], reads=[b_tm], writes=[db("LAT", i)])
                else:
                    ot, b_ot = otr.next()
                    ss, b_ss = ssr.next()
                    kb.act(ot[:], tm[:], AF.Square, [b_tm], [b_ot, b_ss], scale=1.0 / 32.0, accum_out=ss[:, 0:1])
                    kb.rstd(ss[:, 1:2], ss[:, 0:1], b_ss)
                    kb.stt("dve", ot[:], tm[:], ss[:, 1:2], gbc[:, 4, :], ALU.mult, ALU.mult, [b_tm, b_ss, b_gbc], [b_ot])
                    kb.dma(OUT[(i - 2) * 128:(i - 1) * 128, :], ot[:], reads=[b_ot], writes=[db("OUT")])
        kb.P.barrier()


def _consts():
    s = np.arange(128)[:, None]
    t = np.arange(128)[None, :]
    same = (s // 64) == (t // 64)
    c = {}
    c["k_ident"] = np.eye(128, dtype=np.float32)
    c["k_uf"] = np.where(same & (s <= t), -1.0 / 16.0, 0.0).astype(np.float32)
    c["k_ub"] = np.where(same & (s >= t), -1.0 / 16.0, 0.0).astype(np.float32)
    c["k_amf"] = (same & (s <= t)).astype(np.float32)
    c["k_amb"] = (same & (s >= t)).astype(np.float32)
    a = np.zeros((2, 128), np.float32)
    a[0, :64] = 1.0
    a[1, 64:] = 1.0
    b = np.zeros((2, 128), np.float32)
    b[0, 64:] = -200.0
    b[1, :64] = -200.0
    c["k_mba"], c["k_mbb"] = a, b
    j = np.arange(64)
    s8 = np.arange(8)
    nf = np.concatenate([j + 1, -s8 - 1, 63 - j, [64]]).astype(np.float32)
    nb = np.concatenate([8 * (j // 8) + 8 - (j % 8), s8 - 8, j, [64]]).astype(np.float32)
    c["k_nvec"] = np.stack([nf, nb])
    slo = (np.arange(128) // 16)[:, None]
    tlo = (np.arange(128) // 16)[None, :]
    c["k_mge"] = (tlo >= slo).astype(np.float32)
    c["k_mle"] = (tlo <= slo).astype(np.float32)
    return c


_SWAP = [("gla_lr_f", "gla_lr_b"), ("gla_bias_f", "gla_bias_b"), ("s5_a_re_f", "s5_a_re_b"),
         ("s5_a_im_f", "s5_a_im_b"), ("s5_log_dt_f", "s5_log_dt_b")]
_WNAMES = ["w_mod", "b_mod", "norm1_w", "norm2_w", "final_norm_w", "w_in", "gla_lr_f", "gla_lr_b", "gla_bias_f",
           "gla_bias_b", "gla_norm_w", "s5_a_re_f", "s5_a_im_f", "s5_log_dt_f", "s5_a_re_b", "s5_a_im_b", "s5_log_dt_b",
           "s5_b_re", "s5_b_im", "s5_c_re", "s5_c_im", "s5_d", "s5_w_glu", "s5_b_glu", "w_gla_proj", "w_s5_proj",
           "w_out", "ffn_w_gate", "ffn_w_up", "ffn_w_down", "moe_router", "moe_w_gate", "moe_w_up", "moe_w_down"]


def prep_inputs(inputs):
    f = lambda a: np.ascontiguousarray(np.asarray(a, dtype=np.float32))
    W = {n: f(inputs[n]) for n in _WNAMES}
    Wodd = dict(W)
    for a, b in _SWAP:
        Wodd[a], Wodd[b] = W[b], W[a]
    wi = W["w_in"].copy()
    wi[:, :, OZF:OZF + 16] = W["w_in"][:, :, OZB:OZB + 16]
    wi[:, :, OZB:OZB + 16] = W["w_in"][:, :, OZF:OZF + 16]
    Wodd["w_in"] = wi
    cst = _consts()
    x, ctx, c, c_ctx = f(inputs["x"]), f(inputs["ctx"]), f(inputs["c"]), f(inputs["c_ctx"])
    maps = []
    for core in range(8):
        b, p = core // 2, core % 2
        m = dict(Wodd if p else W)
        m.update(cst)
        xb, cb = x[b], ctx[b]
        if p:
            xb, cb = xb[::-1], cb[::-1]
        m["x"] = np.ascontiguousarray(xb)
        m["ctx"] = np.ascontiguousarray(cb)
        cc = np.stack([c[b], c_ctx], axis=-1).reshape(KT, 128, 2).transpose(1, 0, 2)
        m["cc"] = np.ascontiguousarray(cc)
        maps.append(m)
    return maps


_NC_CACHE = {}


def kernel(**inputs):
    if "kb" not in _NC_CACHE:
        _NC_CACHE["kb"] = build()
    kb = _NC_CACHE["kb"]
    maps = prep_inputs(inputs)
    maps = [{k: v for k, v in m.items() if k in kb.ins} for m in maps]
    res = run_bass_kernel_spmd(kb.nc, maps, core_ids=list(range(8)))
    out = np.empty((4, NLAT, D), np.float32)
    for core in range(8):
        b, p = core // 2, core % 2
        o = np.asarray(res.results[core]["out"], dtype=np.float32)
        if p:
            out[b, NLAT - NOWN:] = o[::-1]
        else:
            out[b, :NOWN] = o
    return out
```
